# Optimizing a Trainium2 kernel written in Bass

```python
import math
import jax, jax.numpy as jnp
from jax import lax
import numpy as np

D_MODEL = 1024
BATCH = 32
SEQ = 2048
DEPTH = 1

HEAD_DIM = 64
MOBA_HEADS = 8
MOBA_WIDTH = MOBA_HEADS * HEAD_DIM
MOBA_BLOCK = 256
MOBA_TOPK = 3
MOBA_QCHUNK = 128
DIFF_HEADS = 4
DIFF_QK_DIM = HEAD_DIM
DIFF_V_DIM = 2 * HEAD_DIM
DIFF_WIDTH = DIFF_HEADS * DIFF_V_DIM
MIX_WIDTH = MOBA_WIDTH + DIFF_WIDTH
DIFF_QK_COLS = DIFF_HEADS * 2 * DIFF_QK_DIM
IN_COLS = 3 * MOBA_WIDTH + 2 * DIFF_QK_COLS + DIFF_WIDTH
ATTN_QBLOCK = 128
MEM_LEN = 256
MEM_HEADS = 4
MEM_HEAD_DIM = D_MODEL // MEM_HEADS
N_GROUPS = 4
EXPERTS_PER_GROUP = 8
N_EXPERTS = N_GROUPS * EXPERTS_PER_GROUP
TOPK_IN_GROUP = 2
EXPERT_FF = D_MODEL // 2
RMS_EPS = 1e-6
NEG_INF = -1e30

kernel_name = 'hymba_moba_diff_hmoe_block'


def _rmsnorm(x, g):
    xf = x.astype(jnp.float32)
    y = xf * lax.rsqrt(jnp.mean(xf * xf, axis=-1, keepdims=True) + RMS_EPS)
    return (y * g.astype(jnp.float32)).astype(x.dtype)


def _alibi_slopes(n):
    return jnp.asarray(np.array([2.0 ** (-8.0 * (i + 1) / n) for i in range(n)], dtype=np.float32))


def _lambda_init(layer):
    return 0.8 - 0.6 * math.exp(-0.3 * layer)


def _moba_attention(q, k, v):
    B, S, H, D = q.shape
    L = MOBA_BLOCK
    QC = MOBA_QCHUNK
    nblk = -(-S // L)
    s_pad = nblk * L
    nchunk = S // QC
    n_sel = min(MOBA_TOPK, nblk)
    scale = D ** -0.5
    slopes = _alibi_slopes(H)
    pad = ((0, 0), (0, s_pad - S), (0, 0), (0, 0))
    kb = jnp.pad(k, pad).reshape(B, nblk, L, H, D).transpose(0, 3, 1, 2, 4)
    vb = jnp.pad(v, pad).reshape(B, nblk, L, H, D).transpose(0, 3, 1, 2, 4)
    k_mean = jnp.mean(kb.astype(jnp.float32), axis=3)
    gate = jnp.einsum('bshd,bhnd->bhsn', q.astype(jnp.float32), k_mean)
    q_blk = jnp.arange(S) // L
    fully_past = jnp.arange(nblk)[None, :] < q_blk[:, None]
    gate = jnp.where(fully_past[None, None], gate, NEG_INF)
    _, sel = lax.top_k(gate, n_sel)
    q_items = q.reshape(B, nchunk, QC, H, D).transpose(0, 1, 3, 2, 4).reshape(B * nchunk, H, QC, D)
    sel_items = sel.reshape(B, H, nchunk, QC, n_sel).transpose(0, 2, 1, 3, 4).reshape(B * nchunk, H, QC, n_sel)
    b_items = jnp.repeat(jnp.arange(B, dtype=jnp.int32), nchunk)
    c_items = jnp.tile(jnp.arange(nchunk, dtype=jnp.int32), B)
    offs = jnp.arange(L)
    h_idx = jnp.arange(H)[:, None, None]
    slot = jnp.arange(n_sel)

    def one_chunk(item):
        qc, sc, bi, ci = item
        kbb = kb[bi]
        vbb = vb[bi]
        t = ci * QC + jnp.arange(QC)
        own = (ci * QC) // L
        k_g = kbb[h_idx, sc]
        v_g = vbb[h_idx, sc]
        k_own = lax.dynamic_index_in_dim(kbb, own, axis=1, keepdims=False)
        v_own = lax.dynamic_index_in_dim(vbb, own, axis=1, keepdims=False)
        s_g = jnp.einsum('hqd,hqnld->hqnl', qc, k_g, preferred_element_type=jnp.float32) * scale
        s_own = jnp.einsum('hqd,hld->hql', qc, k_own, preferred_element_type=jnp.float32) * scale
        dist_g = (t[None, :, None, None] - (sc[..., None] * L + offs)).astype(jnp.float32)
        dist_own = (t[:, None] - (own * L + offs)[None, :]).astype(jnp.float32)
        s_g = s_g - slopes[:, None, None, None] * dist_g
        s_own = s_own - slopes[:, None, None] * dist_own[None]
        s_g = jnp.where((slot < own)[None, None, :, None], s_g, NEG_INF)
        s_own = jnp.where(dist_own[None] >= 0, s_own, NEG_INF)
        scores = jnp.concatenate([s_g.reshape(H, QC, n_sel * L), s_own], axis=-1)
        p = jax.nn.softmax(scores, axis=-1)
        p_g = p[..., :n_sel * L].reshape(H, QC, n_sel, L)
        p_own = p[..., n_sel * L:]
        o = (jnp.einsum('hqnl,hqnld->hqd', p_g, v_g.astype(jnp.float32))
             + jnp.einsum('hql,hld->hqd', p_own, v_own.astype(jnp.float32)))
        return o.astype(qc.dtype)

    o = lax.map(one_chunk, (q_items, sel_items, b_items, c_items))
    return o.reshape(B, nchunk, H, QC, D).transpose(0, 1, 3, 2, 4).reshape(B, S, H * D)


def _diff_attention(q, k, v, lam, subln_w, lam_init):
    B, S, H, _, Dq = q.shape
    QB = ATTN_QBLOCK
    nqb = S // QB
    scale = Dq ** -0.5
    slopes = _alibi_slopes(H)
    k_pos = jnp.arange(S)
    vf = v.astype(jnp.float32)
    q_blocks = q.reshape(B, nqb, QB, H, 2, Dq).transpose(1, 0, 2, 3, 4, 5)

    def one_block(item):
        qb, i = item
        t = i * QB + jnp.arange(QB)
        dist = (t[:, None] - k_pos[None, :]).astype(jnp.float32)
        s = jnp.einsum('bqhcd,bkhcd->bhcqk', qb, k, preferred_element_type=jnp.float32) * scale
        s = s - slopes[None, :, None, None, None] * dist
        s = jnp.where(dist >= 0, s, NEG_INF)
        p = jax.nn.softmax(s, axis=-1)
        a = p[:, :, 0] - lam * p[:, :, 1]
        return jnp.einsum('bhqk,bkhd->bqhd', a, vf)

    o = lax.map(one_block, (q_blocks, jnp.arange(nqb)))
    o = o.transpose(1, 0, 2, 3, 4).reshape(B, S, H, -1)
    o = _rmsnorm(o, subln_w) * (1.0 - lam_init)
    return o.reshape(B, S, -1).astype(v.dtype)


def _memory_cross_attention(xn, memn, w_q, w_kv, w_o):
    B, S, _ = xn.shape
    M = memn.shape[1]
    q = (xn @ w_q).reshape(B, S, MEM_HEADS, MEM_HEAD_DIM)
    kv = (memn @ w_kv).reshape(B, M, 2, MEM_HEADS, MEM_HEAD_DIM)
    s = jnp.einsum('bshd,bmhd->bhsm', q, kv[:, :, 0], preferred_element_type=jnp.float32) * MEM_HEAD_DIM ** -0.5
    p = jax.nn.softmax(s, axis=-1)
    o = jnp.einsum('bhsm,bmhd->bshd', p, kv[:, :, 1].astype(jnp.float32))
    return o.reshape(B, S, D_MODEL).astype(xn.dtype) @ w_o


def _hierarchical_moe(xn, w_rg, b_rg, w_re, b_re, w_gate, w_up, w_down):
    B, S, D = xn.shape
    xf = xn.astype(jnp.float32)
    g_logits = xf @ w_rg.astype(jnp.float32) + b_rg.astype(jnp.float32)
    g_prob = jax.nn.softmax(g_logits, axis=-1)
    g_onehot = jax.nn.one_hot(jnp.argmax(g_logits, axis=-1), N_GROUPS, dtype=jnp.float32)
    g_w = jnp.sum(g_prob * g_onehot, axis=-1, keepdims=True)
    e_logits = (xf @ w_re.astype(jnp.float32) + b_re.astype(jnp.float32)).reshape(B, S, N_GROUPS, EXPERTS_PER_GROUP)
    e_in = jnp.sum(e_logits * g_onehot[..., None], axis=2)
    top_v, top_i = lax.top_k(e_in, TOPK_IN_GROUP)
    top_w = jax.nn.softmax(top_v, axis=-1)
    within = jnp.sum(jax.nn.one_hot(top_i, EXPERTS_PER_GROUP, dtype=jnp.float32) * top_w[..., None], axis=-2)
    combine = (g_onehot[..., :, None] * within[..., None, :] * g_w[..., None]).reshape(B, S, N_EXPERTS)
    w_down_f = w_down.astype(jnp.float32)

    def per_row(item):
        xr, cr = item
        hg = jnp.einsum('sd,edf->sef', xr, w_gate, preferred_element_type=jnp.float32)
        hu = jnp.einsum('sd,edf->sef', xr, w_up, preferred_element_type=jnp.float32)
        hh = jax.nn.silu(hg) * hu * cr[..., None]
        return jnp.einsum('sef,efd->sd', hh, w_down_f).astype(xr.dtype)

    return lax.map(per_row, (xn, combine))


def setup_inputs(seed: int = 0) -> dict:
    key = jax.random.key(seed)
    ks = jax.random.split(key, 25)
    f32 = jnp.float32

    def nrm(k, shape, scale):
        return jax.random.normal(k, shape, f32) * scale

    def gain(k, shape):
        return 1.0 + 0.02 * jax.random.normal(k, shape, f32)

    L = DEPTH
    D = D_MODEL
    return {
        'x': nrm(ks[0], (BATCH, SEQ, D), 1.0),
        'mem': nrm(ks[1], (BATCH, MEM_LEN, D), 1.0),
        'norm_mix': gain(ks[2], (L, D)),
        'w_in': nrm(ks[3], (L, D, IN_COLS), D ** -0.5),
        'lambda_q1': nrm(ks[4], (L, DIFF_QK_DIM), 0.1),
        'lambda_k1': nrm(ks[5], (L, DIFF_QK_DIM), 0.1),
        'lambda_q2': nrm(ks[6], (L, DIFF_QK_DIM), 0.1),
        'lambda_k2': nrm(ks[7], (L, DIFF_QK_DIM), 0.1),
        'diff_subln': gain(ks[8], (L, DIFF_V_DIM)),
        'norm_moba_out': gain(ks[9], (L, MOBA_WIDTH)),
        'w_out': nrm(ks[10], (L, MIX_WIDTH, D), MIX_WIDTH ** -0.5),
        'norm_mem_q': gain(ks[11], (L, D)),
        'norm_mem_kv': gain(ks[12], (L, D)),
        'w_mem_q': nrm(ks[13], (L, D, D), D ** -0.5),
        'w_mem_kv': nrm(ks[14], (L, D, 2 * D), D ** -0.5),
        'w_mem_o': nrm(ks[15], (L, D, D), D ** -0.5),
        'norm_ffn': gain(ks[16], (L, D)),
        'w_router_group': nrm(ks[17], (L, D, N_GROUPS), D ** -0.5),
        'b_router_group': nrm(ks[18], (L, N_GROUPS), 0.01),
        'w_router_expert': nrm(ks[19], (L, D, N_EXPERTS), D ** -0.5),
        'b_router_expert': nrm(ks[20], (L, N_EXPERTS), 0.01),
        'w_expert_gate': nrm(ks[21], (L, N_EXPERTS, D, EXPERT_FF), D ** -0.5),
        'w_expert_up': nrm(ks[22], (L, N_EXPERTS, D, EXPERT_FF), D ** -0.5),
        'w_expert_down': nrm(ks[23], (L, N_EXPERTS, EXPERT_FF, D), EXPERT_FF ** -0.5),
        'norm_final': gain(ks[24], (D,)),
    }


def reference(x, mem, norm_mix, w_in, lambda_q1, lambda_k1, lambda_q2, lambda_k2, diff_subln,
              norm_moba_out, w_out, norm_mem_q, norm_mem_kv, w_mem_q, w_mem_kv, w_mem_o,
              norm_ffn, w_router_group, b_router_group, w_router_expert, b_router_expert,
              w_expert_gate, w_expert_up, w_expert_down, norm_final):
    B, S, _ = x.shape
    split_at = [MOBA_WIDTH, 2 * MOBA_WIDTH, 3 * MOBA_WIDTH,
                3 * MOBA_WIDTH + DIFF_QK_COLS, 3 * MOBA_WIDTH + 2 * DIFF_QK_COLS]
    h = x
    for l in range(DEPTH):
        hn = _rmsnorm(h, norm_mix[l])
        proj = hn @ w_in[l]
        qa, ka, va, qd, kd, vd = jnp.split(proj, split_at, axis=-1)
        a_out = _moba_attention(qa.reshape(B, S, MOBA_HEADS, HEAD_DIM),
                                ka.reshape(B, S, MOBA_HEADS, HEAD_DIM),
                                va.reshape(B, S, MOBA_HEADS, HEAD_DIM))
        a_out = _rmsnorm(a_out, norm_moba_out[l])
        lam_init = _lambda_init(l)
        lam = (jnp.exp(jnp.sum(lambda_q1[l].astype(jnp.float32) * lambda_k1[l].astype(jnp.float32)))
               - jnp.exp(jnp.sum(lambda_q2[l].astype(jnp.float32) * lambda_k2[l].astype(jnp.float32)))
               + lam_init)
        d_out = _diff_attention(qd.reshape(B, S, DIFF_HEADS, 2, DIFF_QK_DIM),
                                kd.reshape(B, S, DIFF_HEADS, 2, DIFF_QK_DIM),
                                vd.reshape(B, S, DIFF_HEADS, DIFF_V_DIM),
                                lam, diff_subln[l], lam_init)
        h = h + jnp.concatenate([a_out, d_out], axis=-1) @ w_out[l]
        h = h + _memory_cross_attention(_rmsnorm(h, norm_mem_q[l]), _rmsnorm(mem, norm_mem_kv[l]),
                                        w_mem_q[l], w_mem_kv[l], w_mem_o[l])
        h = h + _hierarchical_moe(_rmsnorm(h, norm_ffn[l]), w_router_group[l], b_router_group[l],
                                  w_router_expert[l], b_router_expert[l],
                                  w_expert_gate[l], w_expert_up[l], w_expert_down[l])
    return _rmsnorm(h, norm_final)
```

```python
import math
from contextlib import ExitStack

import numpy as np
import ml_dtypes
import concourse.bass as bass
import concourse.mybir as mybir
from concourse.bass_utils import run_bass_kernel_spmd

F32 = mybir.dt.float32
BF16 = mybir.dt.bfloat16
I32 = mybir.dt.int32
AF = mybir.ActivationFunctionType
ALU = mybir.AluOpType
AX = mybir.AxisListType

ENGS = ("sync", "scalar", "gpsimd", "vector", "tensor")
EPOCH = 8000
NCORES = 8
SEQ = 2048
D = 1024
NTOK = 4 * SEQ
NTILE = NTOK // 128
TSLOT = 256
NS = 96
EPS = 1e-6
NEG = -30000.0
SBUF_BYTES = 207872
PE_SMALL_PEN = 40
P3_WARM = 0
P3_WARM_PEN = 400
P3_LAG = 0
P2_WARM = 256
SCHED = "all"


class Buf:
    def __init__(self, name):
        self.name = name
        self.last_w = None
        self.readers = []
        self.sem = None
        self.cnt = 0
        self.gen_deps = []
        self.ssem = None
        self.scnt = 0
        self.group = []


class Op:
    __slots__ = ("eng", "fn", "deps", "needed", "sem", "val", "dma", "wbuf", "alldeps", "idx", "phase", "fin", "busy", "lat", "pen", "store", "sbuf")

    def __init__(self, eng, fn, dma, wbuf):
        self.eng = eng
        self.fn = fn
        self.deps = []
        self.alldeps = []
        self.needed = False
        self.sem = None
        self.val = 0
        self.dma = dma
        self.wbuf = wbuf
        self.idx = 0
        self.phase = 0
        self.fin = None
        self.pen = 0
        self.store = False
        self.sbuf = None


class _Probe:
    def __init__(self):
        self.rec = None

    def __getattr__(self, name):
        def f(*a, **kw):
            self.rec = (name, a, kw)
            return self
        return f


def _est(op):
    pr = _Probe()
    op.fn(pr)
    name, a, kw = pr.rec
    out = kw.get("out", a[0] if a else None)

    def fs(ap):
        sh = ap.shape
        n_ = 1
        for v in sh[1:]:
            n_ *= int(v)
        return n_
    n = fs(out)
    for k in ("in_", "in0"):
        if k in kw and kw[k] is not None:
            try:
                n = max(n, fs(kw[k]))
            except Exception:
                pass
    if name in ("matmul", "transpose"):
        t = fs(out) / 2400.0 + 0.02
        return t, t + 0.16
    esz = 2 if out.dtype == BF16 else 4
    if name == "dma_start":
        nb = fs(out) * int(out.shape[0]) * esz
        return (0.6 if op.eng == "gpsimd" else 0.06), 2.0 + nb / 220e3
    if name == "indirect_dma_start":
        nb = min(fs(out) * int(out.shape[0]), fs(kw["in_"]) * int(kw["in_"].shape[0])) * esz
        return 1.0, 3.0 + nb / 220e3
    if op.eng == "scalar":
        t = n * 0.00076 + 0.22
    elif op.eng == "vector":
        t = n * 0.00095 + 0.22
    else:
        t = n * 0.0021 + 0.3
    return t, t


class Prog:
    def __init__(self, nc):
        self.nc = nc
        self.streams = {e: [] for e in ENGS}
        self.sems = []
        self.all_ops = []
        self.bufs = []
        self.pending = {e: [] for e in ENGS}
        self.phase = 0
        self.phase_evs = {}

    def buf(self, name):
        b = Buf(name)
        self.bufs.append(b)
        return b

    def add(self, eng, fn, reads=(), writes=(), dma=False, join=False, after=(), store=False):
        op = Op(eng, fn, dma, writes[0] if (dma and writes) else None)
        op.store = bool(store and dma)
        if dma:
            op.sbuf = reads[0] if op.store else writes[0]
        deps = list(self.pending[eng])
        self.pending[eng] = []
        deps.extend(after)
        for b in reads:
            deps.extend(b.group)
        for b in writes:
            if join and dma:
                deps.extend(b.gen_deps)
                deps.extend(b.readers)
            else:
                g = list(b.readers)
                g.extend(b.group)
                b.gen_deps = g
                deps.extend(g)
        seen = set()
        for d in deps:
            if d is op or id(d) in seen:
                continue
            seen.add(id(d))
            op.alldeps.append(d)
            if d.eng == "tensor" and eng == "tensor" and not d.dma and not dma:
                continue
            op.deps.append(d)
            d.needed = True
        if dma and join and writes and writes[0].last_w is not None and id(writes[0].last_w) not in seen:
            op.alldeps.append(writes[0].last_w)
        op.idx = len(self.all_ops)
        op.phase = self.phase
        for b in reads:
            b.readers.append(op)
        for b in writes:
            if join and dma:
                b.group.append(op)
            else:
                b.group = [op]
            b.last_w = op
            b.readers = []
        self.streams[eng].append(op)
        self.all_ops.append(op)
        return op

    def barrier(self):
        evs = []
        for b in self.bufs:
            if getattr(b, "nobar", False):
                continue
            evs.extend(b.group)
            evs.extend(b.readers)
        for e in ENGS:
            if self.streams[e]:
                last = [o for o in self.streams[e][-4:] if not o.dma]
                evs.extend(last[-1:])
        for e in ENGS:
            self.pending[e] = list(evs)
        self.phase += 1
        self.phase_evs[self.phase] = list(evs)

    def schedule(self, phases=None):
        import heapq
        succ = {}

        def prio(o):
            if PE_SMALL_PEN and o.eng == "tensor" and o.busy < 0.07:
                return o.idx + PE_SMALL_PEN
            return o.idx + o.pen
        for op in self.all_ops:
            op.busy, op.lat = _est(op)
        for op in self.all_ops:
            for d in op.alldeps:
                succ.setdefault(id(d), []).append(op)
        free_at = {e: 0.0 for e in ENGS}
        new_streams = {e: [] for e in ENGS}
        order = []
        tmax = 0.0
        nph = self.phase + 1
        by_phase = [[] for _ in range(nph)]
        for op in self.all_ops:
            by_phase[op.phase].append(op)
        for p in range(nph):
            ops = by_phase[p]
            if phases is not None and p not in phases:
                t = max([tmax] + list(free_at.values()))
                for op in ops:
                    op.fin = t
                    new_streams[op.eng].append(op)
                    order.append(op)
                continue
            t0 = tmax
            indeg = {}
            rdy = {}
            pend = {e: [] for e in ENGS}
            avail = {e: [] for e in ENGS}
            for op in ops:
                c = 0
                r = t0
                for d in op.alldeps:
                    if d.fin is None:
                        c += 1
                    else:
                        r = max(r, d.fin + (0.0 if d.eng == op.eng and not d.dma else 0.15))
                indeg[id(op)] = c
                rdy[id(op)] = r
                if c == 0:
                    heapq.heappush(pend[op.eng], (r, prio(op), op.idx, op))
            for e in ENGS:
                free_at[e] = max(free_at[e], t0)
            left = len(ops)
            while left:
                best = None
                for e in ENGS:
                    T = free_at[e]
                    while pend[e] and pend[e][0][0] <= T:
                        r, i, i2, o = heapq.heappop(pend[e])
                        heapq.heappush(avail[e], (i, i2, o))
                    if avail[e]:
                        cand = (T, e, True)
                    elif pend[e]:
                        cand = (pend[e][0][0], e, False)
                    else:
                        continue
                    if best is None or cand[0] < best[0]:
                        best = cand
                assert best is not None, "scheduler deadlock"
                st_, e, from_avail = best
                if from_avail:
                    i, i2, op = heapq.heappop(avail[e])
                else:
                    r, i, i2, op = heapq.heappop(pend[e])
                free_at[e] = st_ + op.busy
                op.fin = st_ + op.lat
                tmax = max(tmax, op.fin)
                new_streams[e].append(op)
                order.append(op)
                left -= 1
                for q in succ.get(id(op), ()):
                    k = id(q)
                    if k not in indeg:
                        continue
                    rdy[k] = max(rdy[k], op.fin + (0.0 if q.eng == e and not op.dma else 0.15))
                    indeg[k] -= 1
                    if indeg[k] == 0:
                        heapq.heappush(pend[q.eng], (rdy[k], prio(q), q.idx, q))
        for e in ENGS:
            seen_ph = set()
            for op in new_streams[e]:
                if op.phase in seen_ph:
                    continue
                seen_ph.add(op.phase)
                evs = self.phase_evs.get(op.phase)
                if not evs:
                    continue
                have = set(id(d) for d in op.deps)
                for d in evs:
                    if d is op or id(d) in have:
                        continue
                    have.add(id(d))
                    op.deps.append(d)
                    d.needed = True
        self.streams = new_streams
        self.all_ops = order
        self.est_total = tmax

    def emit(self, stack):
        nc = self.nc
        cnts = {e: 0 for e in ENGS}
        curs = {e: None for e in ENGS}
        for op in self.all_ops:
            eng = op.eng
            if op.dma and op.store:
                b = op.sbuf
                if b.ssem is None:
                    b.ssem = stack.enter_context(nc.semaphore("s_" + b.name))
                    self.sems.append(b.ssem)
                b.scnt += 1
                op.sem = b.ssem
                op.val = 16 * b.scnt
            elif op.dma:
                b = op.sbuf
                if b.sem is None:
                    b.sem = stack.enter_context(nc.semaphore("d_" + b.name))
                    self.sems.append(b.sem)
                b.cnt += 1
                op.sem = b.sem
                op.val = 16 * b.cnt
            elif op.needed:
                if curs[eng] is None or cnts[eng] >= EPOCH:
                    curs[eng] = stack.enter_context(nc.semaphore("c_%s_%d" % (eng, len(self.sems))))
                    self.sems.append(curs[eng])
                    cnts[eng] = 0
                cnts[eng] += 1
                op.sem = curs[eng]
                op.val = cnts[eng]
        block = stack.enter_context(nc.Block())
        prog = self

        def run(e, eng):
            waited = {}
            for op in prog.streams[eng]:
                need = {}
                for d in op.deps:
                    k = id(d.sem)
                    if d.val > need.get(k, (None, 0))[1]:
                        need[k] = (d.sem, d.val)
                for k, (sem_, val_) in need.items():
                    if waited.get(k, 0) < val_:
                        e.wait_ge(sem_, val_)
                        waited[k] = val_
                ins = op.fn(e)
                if op.dma:
                    ins.then_inc(op.sem, 16)
                elif op.needed:
                    ins.then_inc(op.sem, 1)
            last = {}
            for op in prog.streams[eng]:
                if op.dma:
                    last[id(op.sem)] = (op.sem, max(op.val, last.get(id(op.sem), (None, 0))[1]))
            for s, v in last.values():
                if waited.get(id(s), 0) < v:
                    e.wait_ge(s, v)

        @block.sync
        def _(e):
            run(e, "sync")

        @block.scalar
        def _(e):
            run(e, "scalar")

        @block.gpsimd
        def _(e):
            run(e, "gpsimd")

        @block.vector
        def _(e):
            run(e, "vector")

        @block.tensor
        def _(e):
            run(e, "tensor")


class Mem:
    def __init__(self, big):
        self.big = big
        self.off = 0

    def alloc(self, shape, dt=BF16):
        esz = 2 if dt == BF16 else 4
        n = int(np.prod(shape[1:])) * esz
        off = self.off
        self.off += (n + 63) // 64 * 64
        assert self.off <= SBUF_BYTES, ("SBUF overflow", self.off)
        ap = self.big[0:shape[0], off // 2:(off + n) // 2]
        if dt != BF16:
            ap = ap.bitcast(dt)
        if len(shape) > 2:
            names = ["d%d" % i for i in range(len(shape) - 1)]
            kw = {nm: int(s) for nm, s in zip(names[1:], shape[2:])}
            ap = ap.rearrange("p (%s) -> p %s" % (" ".join(names), " ".join(names)), **kw)
        return ap


def bc(ap, shape, axis):
    return ap.unsqueeze(axis).to_broadcast(list(shape))


def build(debug=False):
    nc = bass.Bass("TRN2", target_bir_lowering=False)

    def din(name, shape, dt=F32):
        return nc.dram_tensor(name, list(shape), dt, kind="ExternalInput").ap()

    def dscr(name, shape, dt):
        return nc.dram_tensor(name, list(shape), dt, kind=("ExternalOutput" if debug else "Internal")).ap()

    x = din("x", [NTOK, D])
    mem = din("mem", [1024, D])
    w_in = din("w_in", [D, 3072])
    w_out = din("w_out", [D, D])
    w_mem_q = din("w_mem_q", [D, D])
    w_mem_kv = din("w_mem_kv", [D, 2048])
    w_mem_o = din("w_mem_o", [D, D])
    w_rg = din("w_rg", [D, 4])
    w_re = din("w_re", [D, 32])
    b_rg = din("b_rg", [1, 4])
    b_re = din("b_re", [1, 32])
    w_gate = din("w_gate", [32 * D, 512])
    w_up = din("w_up", [32 * D, 512])
    w_down = din("w_down", [32 * 512, D])
    norm_mix = din("norm_mix", [1, D])
    norm_moba = din("norm_moba", [1, 512])
    subln = din("subln", [1, 128])
    norm_mem_q = din("norm_mem_q", [1, D])
    norm_mem_kv = din("norm_mem_kv", [1, D])
    norm_ffn = din("norm_ffn", [1, D])
    norm_final = din("norm_final", [1, D])
    lam4 = din("lam4", [4, 64])
    c_ident = din("c_ident", [128, 128])
    c_identb = din("c_identb", [128, 128], BF16)
    c_tri = din("c_tri", [128, 128], BF16)
    c_ltri = din("c_ltri", [128, 128], BF16)
    c_kmoba = din("c_kmoba", [8, 12, SEQ], BF16)
    c_qmoba = din("c_qmoba", [8, 4, SEQ], BF16)
    c_kdiff = din("c_kdiff", [4, 4, SEQ], BF16)
    c_qdiff = din("c_qdiff", [4, 4, SEQ], BF16)
    c_past = din("c_past", [2, 128])
    c_misc = din("c_misc", [128, 256])
    out = nc.dram_tensor("out", [NTOK, D], F32, kind="ExternalOutput").ap()

    qk_s = dscr("qk_s", [4, 64, 32, SEQ], BF16)
    v_s = dscr("v_s", [4, SEQ, 1040], BF16)
    att_s = dscr("att_s", [NTOK, D], F32)
    h2_s = dscr("h2_s", [NTOK, D], F32)
    xn_s = dscr("xn_s", [NTOK, D], BF16)
    xs_s = dscr("xs_s", [NS * TSLOT, D], BF16)
    y_s = dscr("y_s", [NS * TSLOT, D], BF16)
    dbg_s = dscr("dbg_s", [128, 1024], F32)
    wgu_b = dscr("wgu_b", [32 * 128, 8 * 1024], BF16)
    wd_b = dscr("wd_b", [32 * 128, 4 * 1024], BF16)

    st = ExitStack()
    with st:
        big = st.enter_context(nc.sbuf_tensor("big", [128, SBUF_BYTES // 2], BF16))
        banks = [st.enter_context(nc.psum_tensor("bank%d" % i, [128, 512], F32)) for i in range(8)]
        P = Prog(nc)
        M = Mem(big)
        bankB = [P.buf("bank%d" % i) for i in range(8)]

        def bk(i):
            return banks[i][:]

        def bkb(i):
            return banks[i][:].bitcast(BF16)

        ident_f = M.alloc([128, 128], F32)
        ident_b = M.alloc([128, 128], BF16)
        tri = M.alloc([128, 128], BF16)
        ltri = M.alloc([128, 128], BF16)
        ones_b = M.alloc([128, 128], BF16)
        misc = M.alloc([128, 256], F32)
        past = M.alloc([128, 2, 128], F32)
        eps_t = M.alloc([128, 1], F32)
        lamt = M.alloc([128, 4, 64], F32)
        lams = M.alloc([128, 8], F32)
        A1 = M.alloc([128, NTILE, 32], BF16)
        A2 = M.alloc([128, NTILE, 32], BF16)
        c12 = M.alloc([128, NTILE, 2], F32)
        posi = M.alloc([128, NTILE, 2], I32)
        widx = M.alloc([128, NS], I32)
        Bc = P.buf("consts")
        Blam = P.buf("lam")
        BA = P.buf("Atab")
        Bpos = P.buf("postab")
        first = [True]

        def cload(dst, src, eng="sync"):
            P.add(eng, lambda e: e.dma_start(out=dst, in_=src), writes=[Bc], dma=True, join=not first[0])
            first[0] = False

        cload(ident_f, c_ident[:, :])
        cload(ident_b, c_identb[:, :])
        cload(tri, c_tri[:, :])
        cload(ltri, c_ltri[:, :])
        cload(misc, c_misc[:, :])
        cload(past[:, 0, :], c_past[0:1, :].partition_broadcast(128))
        cload(past[:, 1, :], c_past[1:2, :].partition_broadcast(128))
        for i in range(4):
            cload(lamt[:, i, :], lam4[i:i + 1, :].partition_broadcast(128))
        P.add("vector", lambda e: e.memset(ones_b, 1.0), writes=[Bc])
        P.add("vector", lambda e: e.memset(eps_t, EPS), writes=[Bc])
        lam_init = 0.8 - 0.6 * math.exp(-0.3 * 0)
        ljunk = M.alloc([128, 64], F32)
        for i in range(2):
            P.add("vector", lambda e, i=i: e.tensor_tensor(out=ljunk, in0=lamt[:, 2 * i, :], in1=lamt[:, 2 * i + 1, :], op=ALU.mult),
                  reads=[Bc], writes=[Blam])
            P.add("vector", lambda e, i=i: e.tensor_reduce(out=lams[:, i:i + 1], in_=ljunk, axis=AX.X, op=ALU.add),
                  reads=[Blam], writes=[Blam])
        P.add("scalar", lambda e: e.activation(out=lams[:, 2:4], in_=lams[:, 0:2], func=AF.Exp), reads=[Blam], writes=[Blam])
        P.add("vector", lambda e: e.tensor_tensor(out=lams[:, 4:5], in0=lams[:, 3:4], in1=lams[:, 2:3], op=ALU.subtract),
              reads=[Blam], writes=[Blam])
        P.add("vector", lambda e: e.tensor_scalar(out=lams[:, 4:5], in0=lams[:, 4:5], scalar1=-lam_init, scalar2=None, op0=ALU.add),
              reads=[Blam], writes=[Blam])
        neglam = lams[:, 4:5]
        persist_mark = M.off

        JB = {}

        def jbuf(j):
            k = int(j.offset)
            if k not in JB:
                JB[k] = P.buf("junk%d" % len(JB))
            return JB[k]

        def rms_stats(src, width, ssb, Bsrc, Bss, junk):
            P.add("scalar", lambda e: e.activation(out=junk, in_=src, func=AF.Square, accum_out=ssb[:, 0:1]),
                  reads=[Bsrc], writes=[Bss, jbuf(junk)])
            P.add("scalar", lambda e: e.activation(out=ssb[:, 1:2], in_=ssb[:, 0:1], func=AF.Sqrt, bias=eps_t[:, 0:1], scale=1.0 / width),
                  reads=[Bss, Bc], writes=[Bss])
            P.add("vector", lambda e: e.reciprocal(out=ssb[:, 2:3], in_=ssb[:, 1:2]), reads=[Bss], writes=[Bss])

        def load_w_bf16(dst, src, ncols, B):
            k = 0
            for kc in range(8):
                for c0 in range(0, ncols, 1024):
                    P.add("gpsimd", lambda e, kc=kc, c0=c0: e.dma_start(out=dst[:, kc, c0:c0 + 1024], in_=src[kc * 128:(kc + 1) * 128, c0:c0 + 1024]),
                          writes=[B], dma=True, join=(k > 0))
                    k += 1

        def transposes_bf16(src, Bsrc, bank, dst, Bdst, n=8, evac="scalar"):
            pv = bkb(bank)
            for k in range(n):
                P.add("tensor", lambda e, k=k: e.transpose(pv[:, k * 128:(k + 1) * 128], src[:, k * 128:(k + 1) * 128], ident_b),
                      reads=[Bsrc, Bc], writes=[bankB[bank]])
            src_v = pv[:, 0:n * 128].rearrange("p (k t) -> p k t", k=n)
            if evac == "scalar":
                P.add("scalar", lambda e: e.activation(out=dst, in_=src_v, func=AF.Copy), reads=[bankB[bank]], writes=[Bdst])
            else:
                P.add("vector", lambda e: e.tensor_copy(out=dst, in_=src_v), reads=[bankB[bank]], writes=[Bdst])

        w_in_sb = M.alloc([128, 8, 3072], BF16)
        Bwin = P.buf("w_in")
        load_w_bf16(w_in_sb, w_in, 3072, Bwin)
        gmix = M.alloc([128, D], F32)
        Bg = P.buf("gmix")
        P.add("sync", lambda e: e.dma_start(out=gmix, in_=norm_mix[0:1, :].partition_broadcast(128)), writes=[Bg], dma=True)
        xt = [M.alloc([128, D], F32) for _ in range(2)]
        Bxt = [P.buf("xt%d" % i) for i in range(2)]
        ss1 = [M.alloc([128, 4], F32) for _ in range(2)]
        Bss1 = [P.buf("ss1_%d" % i) for i in range(2)]
        junk = M.alloc([128, D], BF16)
        hn = [M.alloc([128, D], BF16) for _ in range(2)]
        Bhn = [P.buf("hn%d" % i) for i in range(2)]
        hnT = [M.alloc([128, 8, 512], BF16) for _ in range(2)]
        BhnT = [P.buf("hnT%d" % i) for i in range(2)]
        qk_st = [M.alloc([128, 16, 512], BF16) for _ in range(2)]
        Bqkst = [P.buf("qkst%d" % i) for i in range(2)]
        vst = [M.alloc([128, 4, 1040], BF16) for _ in range(2)]
        Bvst = [P.buf("vst%d" % i) for i in range(2)]
        Bqk_s = [P.buf("qk_s%d" % b) for b in range(4)]
        for i in range(2):
            P.add("vector", lambda e, i=i: e.memset(vst[i], 1.0), writes=[Bvst[i]])

        Bwcv = P.buf("wconv")
        Bwcv.nobar = True
        wgu_v = wgu_b.rearrange("(e p) (kc two f) -> e p kc two f", p=128, kc=8, two=2)
        wd_v = wd_b.rearrange("(e p) (fc d) -> e p fc d", p=128, fc=4)
        ncv = [0]

        def conv_expert(E, after):
            for (two, src) in ((0, w_gate), (1, w_up)):
                P.add("gpsimd", lambda e, E=E, two=two, src=src: e.dma_start(out=wgu_v[E, :, :, two, :], in_=src[E * 1024:(E + 1) * 1024, :].rearrange("(kc p) f -> p kc f", p=128)),
                      writes=[Bwcv], dma=True, join=(ncv[0] > 0), after=after)
                ncv[0] += 1
            P.add("gpsimd", lambda e, E=E: e.dma_start(out=wd_v[E], in_=w_down[E * 512:(E + 1) * 512, :].rearrange("(fc p) d -> p fc d", p=128)),
                  writes=[Bwcv], dma=True, join=True, after=after)
            ncv[0] += 1

        NCV1 = 10

        pcols = [k * 128 for k in range(4)] + [512 + k * 128 for k in range(4)] + [1536 + k * 128 for k in range(4)] + [2048 + k * 128 for k in range(4)]
        pscale = [0.125] * 4 + [1.0] * 4 + [0.125] * 4 + [1.0] * 4
        evq = [0]
        lastmm = [None]

        def p1_norm(ck, i):
            s2 = ck % 2
            t = ck * 4 + i
            p2 = t % 2
            P.add("sync", lambda e, t=t, p2=p2: e.dma_start(out=xt[p2], in_=x[t * 128:(t + 1) * 128, :]), writes=[Bxt[p2]], dma=True)
            rms_stats(xt[p2], D, ss1[p2], Bxt[p2], Bss1[p2], junk)
            P.add("vector", lambda e, p2=p2: e.scalar_tensor_tensor(out=hn[p2], in0=xt[p2], scalar=ss1[p2][:, 2:3], in1=gmix, op0=ALU.mult, op1=ALU.mult),
                  reads=[Bxt[p2], Bss1[p2], Bg], writes=[Bhn[p2]])
            transposes_bf16(hn[p2], Bhn[p2], p2, hnT[s2][:, :, i * 128:(i + 1) * 128], BhnT[s2], evac=("scalar" if i % 2 else "vector"))

        def p1_mm(ck):
            b, c = ck // 4, ck % 4
            s2 = ck % 2
            for g in range(16):
                bnk = 2 + g % 4
                for kc in range(8):
                    P.add("tensor", lambda e, g=g, kc=kc, bnk=bnk, s2=s2: e.matmul(bk(bnk), lhsT=w_in_sb[:, kc, pcols[g]:pcols[g] + 128], rhs=hnT[s2][:, kc, :],
                                                                                 start=(kc == 0), stop=(kc == 7)),
                          reads=[Bwin, BhnT[s2]], writes=[bankB[bnk]])
                if evq[0] % 2 == 0:
                    P.add("scalar", lambda e, g=g, bnk=bnk, s2=s2: e.activation(out=qk_st[s2][:, g, :], in_=bk(bnk), func=AF.Copy, scale=pscale[g]),
                          reads=[bankB[bnk]], writes=[Bqkst[s2]])
                else:
                    P.add("vector", lambda e, g=g, bnk=bnk, s2=s2: e.tensor_scalar(out=qk_st[s2][:, g, :], in0=bk(bnk), scalar1=pscale[g], scalar2=None, op0=ALU.mult),
                          reads=[bankB[bnk]], writes=[Bqkst[s2]])
                evq[0] += 1
                if g % 4 == 1 and ck + 1 < 16:
                    p1_norm(ck + 1, g // 4)
            qv = qk_s[b].rearrange("d (k two) s -> d k two s", two=2)
            for hh in range(2):
                P.add("sync", lambda e, c=c, s2=s2, hh=hh, qv=qv: e.dma_start(out=qv[:, :, hh, c * 512:(c + 1) * 512], in_=qk_st[s2][hh * 64:(hh + 1) * 64, :, :]),
                      reads=[Bqkst[s2]], writes=[Bqk_s[b]], dma=True, store=True, join=(c > 0 or hh > 0))
            for i in range(4):
                for hf in range(2):
                    bnk = 6 + hf
                    vc = 1024 if hf == 0 else 2560
                    for kc in range(8):
                        lastmm[0] = P.add("tensor", lambda e, kc=kc, i=i, bnk=bnk, vc=vc, s2=s2: e.matmul(bk(bnk), lhsT=hnT[s2][:, kc, i * 128:(i + 1) * 128], rhs=w_in_sb[:, kc, vc:vc + 512],
                                                                                            start=(kc == 0), stop=(kc == 7)),
                              reads=[Bwin, BhnT[s2]], writes=[bankB[bnk]])
                    dstv = vst[s2][:, i, hf * 520:(hf + 1) * 520].rearrange("p (h c) -> p h c", c=65)[:, :, 0:64]
                    srcv = bk(bnk).rearrange("p (h c) -> p h c", c=64)
                    if hf == 0:
                        P.add("scalar", lambda e, dstv=dstv, srcv=srcv: e.activation(out=dstv, in_=srcv, func=AF.Copy), reads=[bankB[bnk]], writes=[Bvst[s2]])
                    else:
                        P.add("vector", lambda e, dstv=dstv, srcv=srcv: e.tensor_copy(out=dstv, in_=srcv), reads=[bankB[bnk]], writes=[Bvst[s2]])
            P.add("sync", lambda e, b=b, c=c, s2=s2: e.dma_start(out=v_s[b, c * 512:(c + 1) * 512, :].rearrange("(i p) d -> p i d", p=128), in_=vst[s2]),
                  reads=[Bvst[s2]], writes=[Bqk_s[b]], dma=True, store=True, join=True)
            for E in range(NCV1):
                if (E * 16) // NCV1 == ck:
                    conv_expert(E, [lastmm[0]])

        for i in range(4):
            p1_norm(0, i)
        for ck in range(16):
            p1_mm(ck)
        P.barrier()

        M.off = persist_mark
        VS = [M.alloc([128, 16, 1040], BF16) for _ in range(2)]
        BVS = [P.buf("VS%d" % i) for i in range(2)]
        KAm = [M.alloc([76, SEQ], BF16) for _ in range(2)]
        QAm = [M.alloc([76, SEQ], BF16) for _ in range(2)]
        KAd = [M.alloc([68, SEQ], BF16) for _ in range(2)]
        QAd = [M.alloc([68, SEQ], BF16) for _ in range(2)]
        BKAm = [P.buf("KAm%d" % i) for i in range(2)]
        BQAm = [P.buf("QAm%d" % i) for i in range(2)]
        BKAd = [P.buf("KAd%d" % i) for i in range(2)]
        BQAd = [P.buf("QAd%d" % i) for i in range(2)]
        NPT = 8
        PT = [M.alloc([128, 512], BF16) for _ in range(NPT)]
        BPT = [P.buf("PT%d" % i) for i in range(NPT)]
        OT = [M.alloc([65, 512], F32) for _ in range(2)]
        BOT = [P.buf("OT%d" % i) for i in range(2)]
        ATT = M.alloc([128, 16, D], F32)
        BATT = [P.buf("ATT%d" % c) for c in range(4)]
        Batt_s = [P.buf("att_s%d" % b) for b in range(4)]
        gt = [M.alloc([128, 16, 8], F32) for _ in range(4)]
        gm = M.alloc([128, 3, 16], F32)
        Bgt = P.buf("gate_tmp")
        MB = [M.alloc([128, 16, 72], BF16) for _ in range(2)]
        BMB = [P.buf("MB%d" % i) for i in range(2)]
        km = M.alloc([64, 8], F32)
        kmb = [M.alloc([64, 8], BF16) for _ in range(2)]
        Bkm = P.buf("km")
        Bkmb = [P.buf("kmb%d" % i) for i in range(2)]
        rc = M.alloc([128, 8], F32)
        Brc = P.buf("rc")
        ftmp = M.alloc([128, 4, 64], F32)
        Bftmp = P.buf("ftmp")
        for i in range(2):
            P.add("vector", lambda e, i=i: e.memset(MB[i], 0.0), writes=[BMB[i]])
        SB_ = [0, 1, 2]
        MISC = 7

        units = []
        for b in range(4):
            for uu in range(16):
                units.append((b, uu))

        def unit_info(u):
            b, uu = units[u]
            moba = uu < 8
            slot = (uu % 2) if moba else ((uu - 8) % 2)
            return b, uu, moba, slot

        def pro_loads(u):
            b, uu, moba, slot = unit_info(u)
            if uu == 0:
                P.add("sync", lambda e: e.dma_start(out=VS[b % 2], in_=v_s[b, :, :].rearrange("(j p) d -> p j d", p=128)),
                      reads=[Bqk_s[b]], writes=[BVS[b % 2]], dma=True)
            if moba:
                h = uu
                P.add("sync", lambda e: e.dma_start(out=KAm[slot][0:64, :], in_=qk_s[b, :, 8 + h, :]), reads=[Bqk_s[b]], writes=[BKAm[slot]], dma=True)
                P.add("sync", lambda e: e.dma_start(out=KAm[slot][64:76, :], in_=c_kmoba[h, :, :]), writes=[BKAm[slot]], dma=True, join=True)
                P.add("sync", lambda e: e.dma_start(out=QAm[slot][0:64, :], in_=qk_s[b, :, h, :]), reads=[Bqk_s[b]], writes=[BQAm[slot]], dma=True)
                P.add("sync", lambda e: e.dma_start(out=QAm[slot][72:76, :], in_=c_qmoba[h, :, :]), writes=[BQAm[slot]], dma=True, join=True)
            else:
                j = uu - 8
                h = j // 2
                P.add("sync", lambda e: e.dma_start(out=KAd[slot][0:64, :], in_=qk_s[b, :, 24 + j, :]), reads=[Bqk_s[b]], writes=[BKAd[slot]], dma=True)
                P.add("sync", lambda e: e.dma_start(out=KAd[slot][64:68, :], in_=c_kdiff[h, :, :]), writes=[BKAd[slot]], dma=True, join=True)
                P.add("sync", lambda e: e.dma_start(out=QAd[slot][0:64, :], in_=qk_s[b, :, 16 + j, :]), reads=[Bqk_s[b]], writes=[BQAd[slot]], dma=True)
                P.add("sync", lambda e: e.dma_start(out=QAd[slot][64:68, :], in_=c_qdiff[h, :, :]), writes=[BQAd[slot]], dma=True, join=True)

        def pro_compute(u, part):
            b, uu, moba, slot = unit_info(u)
            if not moba:
                return
            g1, g2, ge, g3 = gt
            if part == 0:
                P.add("vector", lambda e: e.tensor_reduce(out=km, in_=KAm[slot][0:64, :].rearrange("p (n l) -> p n l", l=256), axis=AX.X, op=ALU.add),
                      reads=[BKAm[slot]], writes=[Bkm])
                P.add("vector", lambda e: e.tensor_copy(out=kmb[slot], in_=km), reads=[Bkm], writes=[Bkmb[slot]])
                return
            if part == 2:
                for q4 in range(4):
                    for tt in range(4):
                        t = q4 * 4 + tt
                        P.add("tensor", lambda e, t=t, tt=tt: e.matmul(bk(MISC)[0:72, tt * 128:(tt + 1) * 128], lhsT=MB[slot][:, t, :], rhs=ident_b, start=True, stop=True),
                              reads=[BMB[slot], Bc], writes=[bankB[MISC]])
                    P.add("vector", lambda e, q4=q4: e.tensor_copy(out=QAm[slot][64:72, q4 * 512:(q4 + 1) * 512], in_=bk(MISC)[64:72, :]),
                          reads=[bankB[MISC]], writes=[BQAm[slot]])
                return
            for t in range(16):
                P.add("tensor", lambda e, t=t: e.matmul(bk(MISC)[:, t * 8:(t + 1) * 8], lhsT=QAm[slot][0:64, t * 128:(t + 1) * 128], rhs=kmb[slot],
                                                        start=True, stop=True),
                      reads=[BQAm[slot], Bkmb[slot]], writes=[bankB[MISC]])
            gps = bk(MISC)[:, 0:128].rearrange("p (t n) -> p t n", n=8)
            pb = past[:, 0, :].rearrange("p (t n) -> p t n", n=8)
            psel = past[:, 1, :].rearrange("p (t n) -> p t n", n=8)
            P.add("vector", lambda e: e.tensor_tensor(out=g1, in0=gps, in1=pb, op=ALU.add), reads=[bankB[MISC], Bc], writes=[Bgt])
            cur = g1
            for r in range(3):
                P.add("vector", lambda e, cur=cur, r=r: e.tensor_reduce(out=gm[:, r, :], in_=cur, axis=AX.X, op=ALU.max), reads=[Bgt], writes=[Bgt])
                if r < 2:
                    nxt = g2 if r == 0 else g3
                    P.add("vector", lambda e, cur=cur, r=r: e.tensor_tensor(out=ge, in0=cur, in1=bc(gm[:, r, :], [128, 16, 8], 2), op=ALU.is_ge),
                          reads=[Bgt], writes=[Bgt])
                    P.add("vector", lambda e, cur=cur, nxt=nxt: e.scalar_tensor_tensor(out=nxt, in0=ge, scalar=-1e30, in1=cur, op0=ALU.mult, op1=ALU.add),
                          reads=[Bgt], writes=[Bgt])
                    cur = nxt
            P.add("vector", lambda e: e.tensor_tensor(out=ge, in0=g1, in1=bc(gm[:, 2, :], [128, 16, 8], 2), op=ALU.is_lt), reads=[Bgt], writes=[Bgt])
            P.add("vector", lambda e: e.scalar_tensor_tensor(out=MB[slot][:, :, 64:72], in0=ge, scalar=NEG, in1=psel, op0=ALU.mult, op1=ALU.mult),
                  reads=[Bgt, Bc], writes=[BMB[slot]])

        tasks = []
        for u in range(len(units)):
            for c in range(4):
                nj = 4 * c + 4
                for j in range(nj):
                    tasks.append(dict(u=u, c=c, j=j, o=(j - 4 * c if j >= 4 * c else None), first=(j == 0), last=(j == nj - 1),
                                      ufirst=(c == 0 and j == 0), cfirst=(c if j == 0 else None)))
        chunk_ctr = [0]

        def opair(tk):
            return tk["pair"]

        def issue_S(ti):
            tk = tasks[ti]
            u = tk["u"]
            b, uu, moba, slot = unit_info(u)
            if tk["ufirst"]:
                if u == 0:
                    pro_loads(0)
                    for part in range(3):
                        pro_compute(0, part)
                if u + 1 < len(units):
                    pro_loads(u + 1)
            if tk["cfirst"] in (1, 2, 3) and u + 1 < len(units):
                pro_compute(u + 1, tk["cfirst"] - 1)
            KA, QA, BK, BQ, nr = (KAm[slot], QAm[slot], BKAm[slot], BQAm[slot], 76) if moba else (KAd[slot], QAd[slot], BKAd[slot], BQAd[slot], 68)
            sb = SB_[ti % 3]
            c, j = tk["c"], tk["j"]
            lo = 0 if tk["o"] is None else tk["o"] * 128
            tk["sb"] = sb
            tk["lo"] = lo
            sop = P.add("tensor", lambda e: e.matmul(bk(sb)[:, lo:512], lhsT=KA[0:nr, j * 128:(j + 1) * 128], rhs=QA[0:nr, c * 512 + lo:(c + 1) * 512], start=True, stop=True),
                        reads=[BK, BQ], writes=[bankB[sb]])
            if tk["cfirst"] == 1:
                for E in range(NCV1, 32):
                    if ((E - NCV1) * 60) // (32 - NCV1) == u:
                        conv_expert(E, [sop])

        def fin_A(tk, a):
            ob = 3 + 2 * tk["pair"] + a
            P.add("vector", lambda e, a=a, ob=ob: e.tensor_copy(out=OT[a], in_=bk(ob)[0:65, :]), reads=[bankB[ob]], writes=[BOT[a]])

        def fin_B(tk, a):
            u, c = tk["u"], tk["c"]
            b, uu, moba, slot = unit_info(u)
            nacc = 1 if moba else 2
            for qt in range(4):
                P.add("tensor", lambda e, a=a, qt=qt: e.transpose(bk(MISC)[:, qt * 65:(qt + 1) * 65], OT[a][0:65, qt * 128:(qt + 1) * 128], ident_f[0:65, 0:65]),
                      reads=[BOT[a], Bc], writes=[bankB[MISC]])
            pf = bk(MISC)[:, 0:260].rearrange("p (q d) -> p q d", d=65)
            P.add("vector", lambda e, pf=pf: e.reciprocal(out=rc[:, 0:4], in_=pf[:, :, 64]), reads=[bankB[MISC]], writes=[Brc])
            if moba:
                col = uu * 64
            else:
                jj = uu - 8
                col = 512 + (jj // 2) * 128 + a * 64
            dst = ATT[:, c * 4:(c + 1) * 4, col:col + 64]
            if moba or (uu - 8) % 2 == 0:
                P.add("vector", lambda e, pf=pf, dst=dst: e.tensor_tensor(out=dst, in0=pf[:, :, 0:64], in1=bc(rc[:, 0:4], [128, 4, 64], 2), op=ALU.mult),
                      reads=[bankB[MISC], Brc], writes=[BATT[c]])
            else:
                P.add("vector", lambda e: e.tensor_scalar(out=rc[:, 4:8], in0=rc[:, 0:4], scalar1=neglam, scalar2=None, op0=ALU.mult),
                      reads=[Brc, Blam], writes=[Brc])
                P.add("vector", lambda e, pf=pf: e.tensor_tensor(out=ftmp, in0=pf[:, :, 0:64], in1=bc(rc[:, 4:8], [128, 4, 64], 2), op=ALU.mult),
                      reads=[bankB[MISC], Brc], writes=[Bftmp])
                P.add("vector", lambda e, dst=dst: e.tensor_tensor(out=dst, in0=dst, in1=ftmp, op=ALU.add), reads=[Bftmp, BATT[c]], writes=[BATT[c]])
            if uu == 15 and a == nacc - 1:
                P.add("sync", lambda e: e.dma_start(out=att_s[b * SEQ + c * 512:b * SEQ + (c + 1) * 512, :].rearrange("(t p) d -> p t d", p=128), in_=ATT[:, c * 4:(c + 1) * 4, :]),
                      reads=[BATT[c]], writes=[Batt_s[b]], dma=True, store=True, join=(c > 0))

        pend = []

        def age_pending(flush=False):
            for it in list(pend):
                tk0 = it[0]
                moba0 = unit_info(tk0["u"])[2]
                nacc0 = 1 if moba0 else 2
                while True:
                    it[1] += 1
                    if it[1] == 1:
                        for a in range(nacc0):
                            fin_A(tk0, a)
                    elif it[1] == 3:
                        fin_B(tk0, 0)
                        if nacc0 == 1:
                            pend.remove(it)
                            break
                    elif it[1] == 4 and nacc0 == 2:
                        fin_B(tk0, 1)
                        pend.remove(it)
                        break
                    if not flush:
                        break

        LOOK = 2
        issued = 0
        for ti, tk in enumerate(tasks):
            while issued < min(len(tasks), ti + 1 + LOOK):
                issue_S(issued)
                issued += 1
            u, c, j = tk["u"], tk["c"], tk["j"]
            b, uu, moba, slot = unit_info(u)
            if tk["first"]:
                tk["pair"] = chunk_ctr[0] % 2
                chunk_ctr[0] += 1
            else:
                tk["pair"] = tasks[ti - 1]["pair"]
            pair = tk["pair"]
            sb, lo = tk["sb"], tk["lo"]
            r = ti % NPT
            P.add("scalar", lambda e, sb=sb, lo=lo, r=r: e.activation(out=PT[r][:, lo:512], in_=bk(sb)[:, lo:512], func=AF.Exp),
                  reads=[bankB[sb]], writes=[BPT[r]])
            if tk["o"] is not None:
                P.add("gpsimd", lambda e, lo=lo, r=r: e.tensor_tensor(out=PT[r][:, lo:lo + 128], in0=PT[r][:, lo:lo + 128], in1=tri, op=ALU.mult),
                      reads=[BPT[r], Bc], writes=[BPT[r]])
            nacc = 1 if moba else 2
            for a in range(nacc):
                ob = 3 + 2 * pair + a
                if moba:
                    vcol = uu * 65
                else:
                    vcol = 520 + (((uu - 8) // 2) * 2 + a) * 65
                P.add("tensor", lambda e, ob=ob, vcol=vcol, lo=lo, r=r, j=j, b=b, tk=tk: e.matmul(bk(ob)[0:65, lo:512], lhsT=VS[b % 2][:, j, vcol:vcol + 65], rhs=PT[r][:, lo:512],
                                                                                                   start=tk["first"], stop=tk["last"], skip_group_check=True),
                      reads=[BVS[b % 2], BPT[r]], writes=[bankB[ob]])
            if moba and P2_WARM:
                db = 4 if ti % 2 == 0 else 6
                P.add("tensor", lambda e, db=db, b=b: e.matmul(bk(db)[:, 0:P2_WARM], lhsT=ident_b, rhs=VS[b % 2][:, 0, 0:P2_WARM], start=True, stop=True),
                      reads=[Bc, BVS[b % 2]], writes=[bankB[db]])
            age_pending()
            if tk["last"]:
                pend.append([tk, 0])
        while pend:
            age_pending(flush=True)
        P.barrier()

        M.off = persist_mark
        kmemT = M.alloc([128, 4, 8, 256], BF16)
        vmem = M.alloc([128, 4, 2, 1028], BF16)
        Bkmem = P.buf("kmemT")
        Bvmem = P.buf("vmem")
        w_out_sb = M.alloc([128, 8, D], BF16)
        w_q_sb = M.alloc([128, 8, D], BF16)
        w_o_sb = M.alloc([128, 8, D], BF16)
        Bwo, Bwq, Bwmo = P.buf("w_out"), P.buf("w_mq"), P.buf("w_mo")
        p3_mark = M.off
        w_kv_sb = M.alloc([128, 8, 2048], BF16)
        Bwkv = P.buf("w_kv")
        load_w_bf16(w_kv_sb, w_mem_kv, 2048, Bwkv)
        load_w_bf16(w_out_sb, w_out, D, Bwo)
        load_w_bf16(w_q_sb, w_mem_q, D, Bwq)
        load_w_bf16(w_o_sb, w_mem_o, D, Bwmo)
        gkv = M.alloc([128, D], F32)
        Bgkv = P.buf("gkv")
        P.add("sync", lambda e: e.dma_start(out=gkv, in_=norm_mem_kv[0:1, :].partition_broadcast(128)), writes=[Bgkv], dma=True)
        mt_ = [M.alloc([128, D], F32) for _ in range(2)]
        Bmt = [P.buf("mt%d" % i) for i in range(2)]
        ssm = [M.alloc([128, 4], F32) for _ in range(2)]
        Bssm = [P.buf("ssm%d" % i) for i in range(2)]
        mn = [M.alloc([128, D], BF16) for _ in range(2)]
        Bmn = [P.buf("mn%d" % i) for i in range(2)]
        mnT = [M.alloc([128, 8, 256], BF16) for _ in range(2)]
        BmnT = [P.buf("mnT%d" % i) for i in range(2)]
        junk = M.alloc([128, D], BF16)
        P.add("vector", lambda e: e.memset(vmem, 1.0), writes=[Bvmem])
        def p3a_b(b):
            s2 = b % 2
            for mt in range(2):
                t = b * 2 + mt
                p2 = t % 2
                P.add("sync", lambda e, t=t, p2=p2: e.dma_start(out=mt_[p2], in_=mem[t * 128:(t + 1) * 128, :]), writes=[Bmt[p2]], dma=True)
                rms_stats(mt_[p2], D, ssm[p2], Bmt[p2], Bssm[p2], junk)
                P.add("vector", lambda e, p2=p2: e.scalar_tensor_tensor(out=mn[p2], in0=mt_[p2], scalar=ssm[p2][:, 2:3], in1=gkv, op0=ALU.mult, op1=ALU.mult),
                      reads=[Bmt[p2], Bssm[p2], Bgkv], writes=[Bmn[p2]])
                transposes_bf16(mn[p2], Bmn[p2], p2, mnT[s2][:, :, mt * 128:(mt + 1) * 128], BmnT[s2], evac=("scalar" if mt else "vector"))
            for fc in range(8):
                bnk = 2 + fc % 4
                for kc in range(8):
                    P.add("tensor", lambda e, fc=fc, kc=kc, bnk=bnk, s2=s2: e.matmul(bk(bnk)[:, 0:256], lhsT=w_kv_sb[:, kc, fc * 128:(fc + 1) * 128], rhs=mnT[s2][:, kc, :],
                                                                                   start=(kc == 0), stop=(kc == 7)),
                          reads=[Bwkv, BmnT[s2]], writes=[bankB[bnk]])
                if fc % 2:
                    P.add("scalar", lambda e, fc=fc, bnk=bnk, b=b: e.activation(out=kmemT[:, b, fc, :], in_=bk(bnk)[:, 0:256], func=AF.Copy), reads=[bankB[bnk]], writes=[Bkmem])
                else:
                    P.add("vector", lambda e, fc=fc, bnk=bnk, b=b: e.tensor_copy(out=kmemT[:, b, fc, :], in_=bk(bnk)[:, 0:256]), reads=[bankB[bnk]], writes=[Bkmem])
            for mt in range(2):
                for hf in range(2):
                    bnk = 6 + hf
                    for kc in range(8):
                        P.add("tensor", lambda e, kc=kc, mt=mt, hf=hf, bnk=bnk, s2=s2: e.matmul(bk(bnk), lhsT=mnT[s2][:, kc, mt * 128:(mt + 1) * 128],
                                                                                              rhs=w_kv_sb[:, kc, 1024 + hf * 512:1024 + (hf + 1) * 512], start=(kc == 0), stop=(kc == 7)),
                              reads=[Bwkv, BmnT[s2]], writes=[bankB[bnk]])
                    dstv = vmem[:, b, mt, hf * 514:(hf + 1) * 514].rearrange("p (h c) -> p h c", c=257)[:, :, 0:256]
                    srcv = bk(bnk).rearrange("p (h c) -> p h c", c=256)
                    if hf:
                        P.add("scalar", lambda e, dstv=dstv, srcv=srcv: e.activation(out=dstv, in_=srcv, func=AF.Copy), reads=[bankB[bnk]], writes=[Bvmem])
                    else:
                        P.add("vector", lambda e, dstv=dstv, srcv=srcv: e.tensor_copy(out=dstv, in_=srcv), reads=[bankB[bnk]], writes=[Bvmem])

        for b in range(4):
            p3a_b(b)
        P.barrier()

        M.off = p3_mark
        w_r = M.alloc([128, 8, 36], F32)
        b_r = M.alloc([128, 36], F32)
        gmoba = M.alloc([128, 512], F32)
        gsub = M.alloc([128, 128], F32)
        gmq = M.alloc([128, D], F32)
        gffn = M.alloc([128, D], F32)
        Bv3 = P.buf("p3vec")
        vl = [(w_r[:, :, 0:4], w_rg.rearrange("(k p) g -> p k g", p=128)), (w_r[:, :, 4:36], w_re.rearrange("(k p) g -> p k g", p=128)),
              (b_r[:, 0:4], b_rg[0:1, :].partition_broadcast(128)), (b_r[:, 4:36], b_re[0:1, :].partition_broadcast(128)),
              (gmoba, norm_moba[0:1, :].partition_broadcast(128)), (gsub, subln[0:1, :].partition_broadcast(128)),
              (gmq, norm_mem_q[0:1, :].partition_broadcast(128)), (gffn, norm_ffn[0:1, :].partition_broadcast(128))]
        for k, (dst, src) in enumerate(vl):
            P.add("sync", lambda e, dst=dst, src=src: e.dma_start(out=dst, in_=src), writes=[Bv3], dma=True, join=(k > 0))
        P.add("vector", lambda e: e.tensor_scalar(out=gsub, in0=gsub, scalar1=1.0 - lam_init, scalar2=None, op0=ALU.mult), reads=[Bv3], writes=[Bv3])
        NSTR = 2
        TPC = 2
        CW = TPC * 128
        NCH = NTILE // TPC
        RS = range(NSTR)
        at = [M.alloc([128, D], F32) for _ in range(NSTR * TPC)]
        Bat = [P.buf("at%d" % i) for i in range(NSTR * TPC)]
        ssA = [M.alloc([128, 16], F32) for _ in RS]
        BssA = [P.buf("ssA%d" % i) for i in RS]
        mixb = [M.alloc([128, D], BF16) for _ in RS]
        Bmixb = [P.buf("mixb%d" % i) for i in RS]
        TT = [M.alloc([128, 8, CW], BF16) for _ in RS]
        BTT = [[P.buf("TT%d_%d" % (s_, i)) for i in range(TPC)] for s_ in RS]
        hres = [M.alloc([128, TPC, D], F32) for _ in RS]
        Bh = [[P.buf("hres%d_%d" % (s_, i)) for i in range(TPC)] for s_ in RS]
        ssB = [M.alloc([128, 4], F32) for _ in RS]
        BssB = [P.buf("ssB%d" % i) for i in RS]
        qmT = [M.alloc([128, 8, CW], BF16) for _ in RS]
        BqmT = [P.buf("qmT%d" % i) for i in RS]
        PmT = [M.alloc([128, 8, CW], BF16) for _ in RS]
        BPm = [[P.buf("PmT%d_%d" % (s_, i)) for i in range(8)] for s_ in RS]
        ca = [M.alloc([128, TPC, D], BF16) for _ in RS]
        Bca = [[P.buf("ca%d_%d" % (s_, i)) for i in range(TPC)] for s_ in RS]
        rc3 = [M.alloc([128, 4], F32) for _ in RS]
        Brc3 = [P.buf("rc3_%d" % i) for i in RS]
        xf = [M.alloc([128, D], F32) for _ in RS]
        Bxf = [P.buf("xf%d" % i) for i in RS]
        xfT = [M.alloc([128, 8, 128], F32) for _ in RS]
        BxfT = [P.buf("xfT%d" % i) for i in RS]
        junk = M.alloc([128, D], BF16)
        lg = [M.alloc([128, TPC, 36], F32) for _ in RS]
        rt = [M.alloc([128, 192], F32) for _ in RS]
        Brt = [P.buf("rt%d" % i) for i in RS]
        Bh2s = P.buf("h2_s")
        Bxns = P.buf("xn_s")
        mmb = [0]
        nst = [0, 0]

        def nxt_bank():
            bnk = 2 + mmb[0] % (3 if P3_WARM else 4)
            mmb[0] += 1
            return bnk

        def p3_warm():
            for _ in range(P3_WARM):
                o = P.add("tensor", lambda e: e.matmul(bk(5), lhsT=ident_b, rhs=w_out_sb[:, 0, 0:512], start=True, stop=True),
                          reads=[Bc, Bwo], writes=[bankB[5]])
                o.pen = P3_WARM_PEN

        def dense_out(srcT, BsrcT, w_sb, Bw, i, hr, Bhr):
            for hf in range(2):
                bnk = nxt_bank()
                for kc in range(8):
                    P.add("tensor", lambda e, kc=kc, hf=hf, bnk=bnk: e.matmul(bk(bnk), lhsT=srcT[:, kc, i * 128:(i + 1) * 128], rhs=w_sb[:, kc, hf * 512:(hf + 1) * 512],
                                                                               start=(kc == 0), stop=(kc == 7)),
                          reads=[BsrcT, Bw], writes=[bankB[bnk]])
                P.add("vector", lambda e, hf=hf, bnk=bnk: e.tensor_tensor(out=hr[:, hf * 512:(hf + 1) * 512], in0=bk(bnk), in1=hr[:, hf * 512:(hf + 1) * 512], op=ALU.add),
                      reads=[bankB[bnk], Bhr], writes=[Bhr])

        def p3_stage(ck, st_):
            s_ = ck % NSTR
            b = (ck * CW) // SEQ
            if st_ == -1:
                for i in range(TPC):
                    t = ck * TPC + i
                    ai = s_ * TPC + i
                    P.add("sync", lambda e, t=t, ai=ai: e.dma_start(out=at[ai], in_=att_s[t * 128:(t + 1) * 128, :]), reads=[Batt_s[b]], writes=[Bat[ai]], dma=True)
            elif st_ == 0:
                for i in range(TPC):
                    t = ck * TPC + i
                    hr = hres[s_][:, i, :]
                    ai = s_ * TPC + i
                    P.add("sync", lambda e, t=t, hr=hr: e.dma_start(out=hr, in_=x[t * 128:(t + 1) * 128, :]), writes=[Bh[s_][i]], dma=True)
                    sa = ssA[s_]
                    P.add("scalar", lambda e, ai=ai, sa=sa, junk=junk: e.activation(out=junk[:, 0:512], in_=at[ai][:, 0:512], func=AF.Square, accum_out=sa[:, 0:1]),
                          reads=[Bat[ai]], writes=[BssA[s_], jbuf(junk)])
                    for h in range(4):
                        P.add("scalar", lambda e, ai=ai, sa=sa, h=h, junk=junk: e.activation(out=junk[:, 0:128], in_=at[ai][:, 512 + h * 128:512 + (h + 1) * 128], func=AF.Square,
                                                                                 accum_out=sa[:, 1 + h:2 + h]),
                              reads=[Bat[ai]], writes=[BssA[s_], jbuf(junk)])
                    P.add("scalar", lambda e, sa=sa: e.activation(out=sa[:, 5:6], in_=sa[:, 0:1], func=AF.Sqrt, bias=eps_t[:, 0:1], scale=1.0 / 512),
                          reads=[BssA[s_], Bc], writes=[BssA[s_]])
                    P.add("scalar", lambda e, sa=sa: e.activation(out=sa[:, 6:10], in_=sa[:, 1:5], func=AF.Sqrt, bias=eps_t[:, 0:1], scale=1.0 / 128),
                          reads=[BssA[s_], Bc], writes=[BssA[s_]])
                    P.add("vector", lambda e, sa=sa: e.reciprocal(out=sa[:, 10:15], in_=sa[:, 5:10]), reads=[BssA[s_]], writes=[BssA[s_]])
                    P.add("vector", lambda e, ai=ai, sa=sa: e.scalar_tensor_tensor(out=mixb[s_][:, 0:512], in0=at[ai][:, 0:512], scalar=sa[:, 10:11], in1=gmoba, op0=ALU.mult, op1=ALU.mult),
                          reads=[Bat[ai], BssA[s_], Bv3], writes=[Bmixb[s_]])
                    for h in range(4):
                        P.add("vector", lambda e, ai=ai, sa=sa, h=h: e.scalar_tensor_tensor(out=mixb[s_][:, 512 + h * 128:512 + (h + 1) * 128], in0=at[ai][:, 512 + h * 128:512 + (h + 1) * 128],
                                                                                    scalar=sa[:, 11 + h:12 + h], in1=gsub, op0=ALU.mult, op1=ALU.mult),
                              reads=[Bat[ai], BssA[s_], Bv3], writes=[Bmixb[s_]])
                    transposes_bf16(mixb[s_], Bmixb[s_], s_, TT[s_][:, :, i * 128:(i + 1) * 128], BTT[s_][i], evac=("scalar" if i % 2 else "vector"))
                    dense_out(TT[s_], BTT[s_][i], w_out_sb, Bwo, i, hr, Bh[s_][i])
            elif st_ == 1:
                for i in range(TPC):
                    hr = hres[s_][:, i, :]
                    rms_stats(hr, D, ssB[s_], Bh[s_][i], BssB[s_], junk)
                    P.add("vector", lambda e, hr=hr: e.scalar_tensor_tensor(out=mixb[s_], in0=hr, scalar=ssB[s_][:, 2:3], in1=gmq, op0=ALU.mult, op1=ALU.mult),
                          reads=[Bh[s_][i], BssB[s_], Bv3], writes=[Bmixb[s_]])
                    transposes_bf16(mixb[s_], Bmixb[s_], s_, TT[s_][:, :, i * 128:(i + 1) * 128], BTT[s_][i], evac=("scalar" if i % 2 else "vector"))
            elif st_ == 2:
                for fc in range(8):
                    bnk = nxt_bank()
                    for kc in range(8):
                        P.add("tensor", lambda e, fc=fc, kc=kc, bnk=bnk: e.matmul(bk(bnk)[:, 0:CW], lhsT=w_q_sb[:, kc, fc * 128:(fc + 1) * 128], rhs=TT[s_][:, kc, :], start=(kc == 0), stop=(kc == 7)),
                              reads=[Bwq] + BTT[s_], writes=[bankB[bnk]])
                    if fc % 2:
                        P.add("scalar", lambda e, fc=fc, bnk=bnk: e.activation(out=qmT[s_][:, fc, :], in_=bk(bnk)[:, 0:CW], func=AF.Copy, scale=1.0 / 16), reads=[bankB[bnk]], writes=[BqmT[s_]])
                    else:
                        P.add("vector", lambda e, fc=fc, bnk=bnk: e.tensor_scalar(out=qmT[s_][:, fc, :], in0=bk(bnk)[:, 0:CW], scalar1=1.0 / 16, scalar2=None, op0=ALU.mult),
                              reads=[bankB[bnk]], writes=[BqmT[s_]])
            elif st_ == 3:
                for h in range(4):
                    for mt in range(2):
                        bnk = nxt_bank()
                        for hf in range(2):
                            P.add("tensor", lambda e, h=h, mt=mt, hf=hf, bnk=bnk: e.matmul(bk(bnk)[:, 0:CW], lhsT=kmemT[:, b, h * 2 + hf, mt * 128:(mt + 1) * 128], rhs=qmT[s_][:, h * 2 + hf, :],
                                                                                            start=(hf == 0), stop=(hf == 1)),
                                  reads=[Bkmem, BqmT[s_]], writes=[bankB[bnk]])
                        P.add("scalar", lambda e, h=h, mt=mt, bnk=bnk: e.activation(out=PmT[s_][:, h * 2 + mt, :], in_=bk(bnk)[:, 0:CW], func=AF.Exp), reads=[bankB[bnk]], writes=[BPm[s_][h * 2 + mt]])
            elif st_ == 4:
                for i in range(TPC):
                    for h in range(4):
                        bnk = nxt_bank()
                        for mt in range(2):
                            P.add("tensor", lambda e, i=i, h=h, mt=mt, bnk=bnk: e.matmul(bk(bnk)[:, 0:257], lhsT=PmT[s_][:, h * 2 + mt, i * 128:(i + 1) * 128], rhs=vmem[:, b, mt, h * 257:(h + 1) * 257],
                                                                                          start=(mt == 0), stop=(mt == 1)),
                                  reads=[BPm[s_][h * 2 + mt], Bvmem], writes=[bankB[bnk]])
                        P.add("vector", lambda e, h=h, bnk=bnk: e.reciprocal(out=rc3[s_][:, h:h + 1], in_=bk(bnk)[:, 256:257]), reads=[bankB[bnk]], writes=[Brc3[s_]])
                        P.add("vector", lambda e, i=i, h=h, bnk=bnk: e.tensor_scalar(out=ca[s_][:, i, h * 256:(h + 1) * 256], in0=bk(bnk)[:, 0:256], scalar1=rc3[s_][:, h:h + 1], scalar2=None, op0=ALU.mult),
                              reads=[bankB[bnk], Brc3[s_]], writes=[Bca[s_][i]])
            elif st_ == 5:
                for i in range(TPC):
                    transposes_bf16(ca[s_][:, i, :], Bca[s_][i], s_, TT[s_][:, :, i * 128:(i + 1) * 128], BTT[s_][i], evac=("scalar" if i % 2 else "vector"))
                for i in range(TPC):
                    dense_out(TT[s_], BTT[s_][i], w_o_sb, Bwmo, i, hres[s_][:, i, :], Bh[s_][i])
                P.add("gpsimd", lambda e: e.dma_start(out=h2_s[ck * CW:(ck + 1) * CW, :].rearrange("(i p) d -> p i d", p=128), in_=hres[s_]),
                      reads=Bh[s_], writes=[Bh2s], dma=True, store=True, join=(nst[0] > 0))
                nst[0] += 1
            elif st_ == 6:
                lgp = nxt_bank()
                for i in range(TPC):
                    t = ck * TPC + i
                    hr = hres[s_][:, i, :]
                    rms_stats(hr, D, ssB[s_], Bh[s_][i], BssB[s_], junk)
                    P.add("vector", lambda e, hr=hr: e.scalar_tensor_tensor(out=xf[s_], in0=hr, scalar=ssB[s_][:, 2:3], in1=gffn, op0=ALU.mult, op1=ALU.mult),
                          reads=[Bh[s_][i], BssB[s_], Bv3], writes=[Bxf[s_]])
                    P.add("scalar", lambda e: e.activation(out=mixb[s_], in_=xf[s_], func=AF.Copy), reads=[Bxf[s_]], writes=[Bmixb[s_]])
                    P.add("gpsimd", lambda e, t=t: e.dma_start(out=xn_s[t * 128:(t + 1) * 128, :], in_=mixb[s_]), reads=[Bmixb[s_]], writes=[Bxns], dma=True, store=True, join=(nst[1] > 0))
                    nst[1] += 1
                    for hf in range(2):
                        tb = 6 + hf
                        for k4 in range(4):
                            kc = hf * 4 + k4
                            P.add("tensor", lambda e, kc=kc, k4=k4, tb=tb: e.transpose(bk(tb)[:, k4 * 128:(k4 + 1) * 128], xf[s_][:, kc * 128:(kc + 1) * 128], ident_f),
                                  reads=[Bxf[s_], Bc], writes=[bankB[tb]])
                        srcv = bk(tb).rearrange("p (k t) -> p k t", k=4)
                        if hf:
                            P.add("scalar", lambda e, srcv=srcv: e.activation(out=xfT[s_][:, 4:8, :], in_=srcv, func=AF.Copy), reads=[bankB[tb]], writes=[BxfT[s_]])
                        else:
                            P.add("vector", lambda e, srcv=srcv: e.tensor_copy(out=xfT[s_][:, 0:4, :], in_=srcv), reads=[bankB[tb]], writes=[BxfT[s_]])
                    for kc in range(8):
                        P.add("tensor", lambda e, i=i, kc=kc: e.matmul(bk(lgp)[:, i * 36:(i + 1) * 36], lhsT=xfT[s_][:, kc, :], rhs=w_r[:, kc, :], start=(kc == 0), stop=(kc == 7)),
                              reads=[BxfT[s_], Bv3], writes=[bankB[lgp]])
                P.add("vector", lambda e: e.tensor_tensor(out=lg[s_], in0=bk(lgp)[:, 0:36 * TPC].rearrange("p (t c) -> p t c", c=36), in1=bc(b_r, [128, TPC, 36], 1), op=ALU.add),
                      reads=[bankB[lgp], Bv3, Brt[s_]], writes=[Brt[s_]])
            if st_ == 7:
                T_ = TPC
                off = [0]

                def R(*shape):
                    n = int(np.prod(shape))
                    ap = rt[s_][:, off[0]:off[0] + n]
                    off[0] += n
                    if len(shape) == 2:
                        ap = ap.rearrange("p (a b) -> p a b", b=shape[1])
                    elif len(shape) == 3:
                        ap = ap.rearrange("p (a b c) -> p a b c", b=shape[1], c=shape[2])
                    return ap
                lgs = lg[s_]
                gl = lgs[:, :, 0:4]
                el = lgs[:, :, 4:36].rearrange("p t (g i) -> p t g i", i=8)
                gmax, goh, gsh, gex, gsum, gw = R(T_), R(T_, 4), R(T_, 4), R(T_, 4), R(T_), R(T_)
                prod = R(T_, 4, 8)
                esel = R(T_, 8)
                m1, eq1, e2, m2, eq2 = R(T_), R(T_, 8), R(T_, 8), R(T_), R(T_, 8)
                dd, ed, den, w1, w2 = R(T_), R(T_), R(T_), R(T_), R(T_)
                assert off[0] <= 192
                V = lambda fn, **kw: P.add("vector", fn, reads=[Brt[s_], Bv3] + kw.get("r", []), writes=[Brt[s_]] + kw.get("w", []))
                V(lambda e: e.tensor_reduce(out=gmax, in_=gl, axis=AX.X, op=ALU.max))
                V(lambda e: e.tensor_tensor(out=goh, in0=gl, in1=bc(gmax, [128, T_, 4], 2), op=ALU.is_ge))
                V(lambda e: e.tensor_tensor(out=gsh, in0=gl, in1=bc(gmax, [128, T_, 4], 2), op=ALU.subtract))
                P.add("scalar", lambda e: e.activation(out=gex, in_=gsh, func=AF.Exp), reads=[Brt[s_]], writes=[Brt[s_]])
                V(lambda e: e.tensor_reduce(out=gsum, in_=gex, axis=AX.X, op=ALU.add))
                V(lambda e: e.reciprocal(out=gw, in_=gsum))
                V(lambda e: e.tensor_tensor(out=prod, in0=el, in1=bc(goh, [128, T_, 4, 8], 3), op=ALU.mult))
                V(lambda e: e.tensor_reduce(out=esel, in_=prod.rearrange("p t g i -> p t i g"), axis=AX.X, op=ALU.add))
                V(lambda e: e.tensor_reduce(out=m1, in_=esel, axis=AX.X, op=ALU.max))
                V(lambda e: e.tensor_tensor(out=eq1, in0=esel, in1=bc(m1, [128, T_, 8], 2), op=ALU.is_ge))
                V(lambda e: e.scalar_tensor_tensor(out=e2, in0=eq1, scalar=-1e30, in1=esel, op0=ALU.mult, op1=ALU.add))
                V(lambda e: e.tensor_reduce(out=m2, in_=e2, axis=AX.X, op=ALU.max))
                V(lambda e: e.tensor_tensor(out=eq2, in0=e2, in1=bc(m2, [128, T_, 8], 2), op=ALU.is_ge))
                V(lambda e: e.tensor_tensor(out=dd, in0=m2, in1=m1, op=ALU.subtract))
                P.add("scalar", lambda e: e.activation(out=ed, in_=dd, func=AF.Exp), reads=[Brt[s_]], writes=[Brt[s_]])
                V(lambda e: e.tensor_scalar(out=den, in0=ed, scalar1=1.0, scalar2=None, op0=ALU.add))
                V(lambda e: e.reciprocal(out=w1, in_=den))
                V(lambda e: e.tensor_tensor(out=w2, in0=ed, in1=w1, op=ALU.mult))
                tsl = slice(ck * T_, (ck + 1) * T_)
                V(lambda e: e.tensor_tensor(out=c12[:, tsl, 0], in0=w1, in1=gw, op=ALU.mult), w=[BA])
                V(lambda e: e.tensor_tensor(out=c12[:, tsl, 1], in0=w2, in1=gw, op=ALU.mult), w=[BA])
                V(lambda e: e.tensor_tensor(out=A1[:, tsl, :].rearrange("p t (g i) -> p t g i", i=8), in0=bc(goh, [128, T_, 4, 8], 3), in1=bc(eq1, [128, T_, 4, 8], 2), op=ALU.mult), w=[BA])
                V(lambda e: e.tensor_tensor(out=A2[:, tsl, :].rearrange("p t (g i) -> p t g i", i=8), in0=bc(goh, [128, T_, 4, 8], 3), in1=bc(eq2, [128, T_, 4, 8], 2), op=ALU.mult), w=[BA])

        NPAIR = NCH // NSTR
        seqs = []
        for s_ in RS:
            sq = [(s_, -1)]
            for m in range(NPAIR):
                ck = m * NSTR + s_
                sq += [(ck, 0), (ck, 1)]
                if m > 0:
                    sq.append((ck - NSTR, 7))
                sq.append((ck, 2))
                if m + 1 < NPAIR:
                    sq.append((ck + NSTR, -1))
                sq += [(ck, 3), (ck, 4), (ck, 5), (ck, 6)]
            sq.append(((NPAIR - 1) * NSTR + s_, 7))
            seqs.append(sq)
        nsq = len(seqs[0])
        for i in range(nsq + P3_LAG):
            if i < nsq:
                p3_stage(*seqs[0][i])
                p3_warm()
            if 0 <= i - P3_LAG < nsq:
                p3_stage(*seqs[1][i - P3_LAG])
                p3_warm()
        P.barrier()

        M.off = persist_mark
        Asum = M.alloc([128, NTILE, 32], BF16)
        cnt = M.alloc([128, NTILE, 32], F32)
        pfx = [M.alloc([128, NTILE, 32], F32) for _ in range(2)]
        rfull = M.alloc([128, NTILE, 32], F32)
        sm = M.alloc([128, 16, 32], F32)
        big3 = M.alloc([128, NS, 32], F32)
        big3b = M.alloc([128, NS, 32], F32)
        Ef = M.alloc([128, NS], F32)
        posf = M.alloc([128, NTILE, 2], F32)
        Bd = P.buf("disp")
        VD = lambda fn, **kw: P.add("vector", fn, reads=[Bd, BA, Bc] + kw.get("r", []), writes=[Bd] + kw.get("w", []))
        VD(lambda e: e.tensor_tensor(out=Asum, in0=A1, in1=A2, op=ALU.add))
        for q in range(4):
            P.add("tensor", lambda e, q=q: e.matmul(bk(q), lhsT=ones_b, rhs=Asum[:, q * 16:(q + 1) * 16, :].rearrange("p t e -> p (t e)"), start=True, stop=True),
                  reads=[Bd, Bc], writes=[bankB[q]])
            P.add("tensor", lambda e, q=q: e.matmul(bk(4 + q), lhsT=ltri, rhs=Asum[:, q * 16:(q + 1) * 16, :].rearrange("p t e -> p (t e)"), start=True, stop=True),
                  reads=[Bd, Bc], writes=[bankB[4 + q]])
            VD(lambda e, q=q: e.tensor_copy(out=cnt[:, q * 16:(q + 1) * 16, :].rearrange("p t e -> p (t e)"), in_=bk(q)), r=[bankB[q]])
            VD(lambda e, q=q: e.tensor_copy(out=rfull[:, q * 16:(q + 1) * 16, :].rearrange("p t e -> p (t e)"), in_=bk(4 + q)), r=[bankB[4 + q]])
        VD(lambda e: e.tensor_copy(out=pfx[0], in_=cnt))
        cur = 0
        sft = 1
        while sft < NTILE:
            VD(lambda e, cur=cur, sft=sft: e.tensor_copy(out=pfx[1 - cur][:, 0:sft, :], in_=pfx[cur][:, 0:sft, :]))
            VD(lambda e, cur=cur, sft=sft: e.tensor_tensor(out=pfx[1 - cur][:, sft:NTILE, :], in0=pfx[cur][:, sft:NTILE, :], in1=pfx[cur][:, 0:NTILE - sft, :], op=ALU.add))
            cur = 1 - cur
            sft *= 2
        incl = pfx[cur]
        excl = pfx[1 - cur]
        VD(lambda e: e.tensor_tensor(out=excl, in0=incl, in1=cnt, op=ALU.subtract))
        ntot = sm[:, 0, :]
        VD(lambda e: e.tensor_copy(out=ntot, in_=incl[:, NTILE - 1, :]))
        thr = misc[:, 0:32]
        eidx = misc[:, 32:64]
        sT = misc[:, 64:64 + NS]
        big_a = big3[:, 0:32, :]
        VD(lambda e: e.tensor_tensor(out=big_a, in0=bc(ntot, [128, 32, 32], 2), in1=bc(thr, [128, 32, 32], 1), op=ALU.is_gt))
        nsl = sm[:, 1, :]
        VD(lambda e: e.tensor_reduce(out=nsl, in_=big_a, axis=AX.X, op=ALU.add))
        sc = [sm[:, 2, :], sm[:, 3, :]]
        VD(lambda e: e.tensor_copy(out=sc[0], in_=nsl))
        cur = 0
        sft = 1
        while sft < 32:
            VD(lambda e, cur=cur, sft=sft: e.tensor_copy(out=sc[1 - cur][:, 0:sft], in_=sc[cur][:, 0:sft]))
            VD(lambda e, cur=cur, sft=sft: e.tensor_tensor(out=sc[1 - cur][:, sft:32], in0=sc[cur][:, sft:32], in1=sc[cur][:, 0:32 - sft], op=ALU.add))
            cur = 1 - cur
            sft *= 2
        inc_e = sc[cur]
        base = sm[:, 4, :]
        endb = sm[:, 5, :]
        VD(lambda e: e.tensor_tensor(out=base, in0=inc_e, in1=nsl, op=ALU.subtract))
        VD(lambda e: e.tensor_scalar(out=base, in0=base, scalar1=float(TSLOT), scalar2=None, op0=ALU.mult))
        VD(lambda e: e.tensor_scalar(out=endb, in0=inc_e, scalar1=float(TSLOT), scalar2=None, op0=ALU.mult))
        VD(lambda e: e.tensor_tensor(out=rfull, in0=rfull, in1=excl, op=ALU.add))
        VD(lambda e: e.tensor_tensor(out=rfull, in0=rfull, in1=bc(base, [128, NTILE, 32], 1), op=ALU.add))
        for k, Ak in enumerate((A1, A2)):
            VD(lambda e, Ak=Ak: e.tensor_tensor(out=cnt, in0=rfull, in1=Ak, op=ALU.mult))
            VD(lambda e, k=k: e.tensor_reduce(out=posf[:, :, k], in_=cnt, axis=AX.X, op=ALU.add))
        VD(lambda e: e.tensor_copy(out=posi, in_=posf), w=[Bpos])
        VD(lambda e: e.tensor_tensor(out=big3, in0=bc(base, [128, NS, 32], 1), in1=bc(sT, [128, NS, 32], 2), op=ALU.is_le))
        VD(lambda e: e.tensor_tensor(out=big3b, in0=bc(endb, [128, NS, 32], 1), in1=bc(sT, [128, NS, 32], 2), op=ALU.is_gt))
        VD(lambda e: e.tensor_tensor(out=big3, in0=big3, in1=big3b, op=ALU.mult))
        VD(lambda e: e.tensor_tensor(out=big3, in0=big3, in1=bc(eidx, [128, NS, 32], 1), op=ALU.mult))
        VD(lambda e: e.tensor_reduce(out=Ef, in_=big3, axis=AX.X, op=ALU.add))
        VD(lambda e: e.tensor_scalar(out=widx, in0=Ef, scalar1=128.0, scalar2=misc[:, 160:161], op0=ALU.mult, op1=ALU.add), w=[Bpos])
        if debug:
            P.add("sync", lambda e: e.dma_start(out=dbg_s[:, 0:128], in_=posf.rearrange("p t k -> p (t k)")), reads=[Bd], writes=[P.buf("dbg")], dma=True)
            P.add("sync", lambda e: e.dma_start(out=dbg_s[:, 128:128 + NS], in_=Ef), reads=[Bd], writes=[P.buf("dbg2")], dma=True)
            P.add("sync", lambda e: e.dma_start(out=dbg_s[:, 256:384], in_=c12.rearrange("p t k -> p (t k)")), reads=[BA], writes=[P.buf("dbg3")], dma=True)
        xg = [M.alloc([128, D], BF16) for _ in range(3)]
        Bxg = [P.buf("xg%d" % i) for i in range(3)]
        Bxs = P.buf("xs_s")
        for t in range(NTILE):
            r3 = t % 3
            P.add("sync", lambda e, t=t, r3=r3: e.dma_start(out=xg[r3], in_=xn_s[t * 128:(t + 1) * 128, :]), reads=[Bxns], writes=[Bxg[r3]], dma=True)
            for k in range(2):
                P.add("gpsimd", lambda e, t=t, r3=r3, k=k: e.indirect_dma_start(out=xs_s[:, :], out_offset=bass.IndirectOffsetOnAxis(ap=posi[:, t, k:k + 1], axis=0),
                                                                                in_=xg[r3], in_offset=None),
                      reads=[Bxg[r3], Bpos], writes=[Bxs], dma=True, store=True, join=(t + k > 0))

        slot_mark = M.off
        Wgu = [M.alloc([128, 8, 1024], BF16) for _ in range(2)]
        Wd = [M.alloc([128, 4, D], BF16) for _ in range(2)]
        BW = [P.buf("W%d" % i) for i in range(2)]
        xsl = [M.alloc([128, 2, D], BF16) for _ in range(3)]
        Bxsl = [P.buf("xsl%d" % i) for i in range(3)]
        xT = [M.alloc([128, 8, 256], BF16) for _ in range(2)]
        BxT = [P.buf("xT%d" % i) for i in range(2)]
        sg = [M.alloc([128, 256], F32) for _ in range(2)]
        Bsg = [P.buf("sg%d" % i) for i in range(2)]
        hT = [M.alloc([128, 4, 256], BF16) for _ in range(2)]
        BhT = [P.buf("hT%d" % i) for i in range(2)]
        ys = [M.alloc([128, 2, D], BF16) for _ in range(2)]
        Bys = [P.buf("ys%d" % i) for i in range(2)]
        Bys_s = P.buf("y_s")

        def slot_loads_x(s):
            s3 = s % 3
            P.add("sync", lambda e: e.dma_start(out=xsl[s3], in_=xs_s[s * TSLOT:(s + 1) * TSLOT, :].rearrange("(i p) d -> p i d", p=128)),
                  reads=[Bxs], writes=[Bxsl[s3]], dma=True)

        def slot_loads(s):
            s2 = s % 2
            P.add("gpsimd", lambda e: e.indirect_dma_start(out=Wgu[s2].rearrange("p k f -> p (k f)"), out_offset=None, in_=wgu_b[:, :],
                                                           in_offset=bass.IndirectOffsetOnAxis(ap=widx[:, s:s + 1], axis=0)),
                  reads=[Bpos, Bwcv], writes=[BW[s2]], dma=True)
            P.add("gpsimd", lambda e: e.indirect_dma_start(out=Wd[s2].rearrange("p k f -> p (k f)"), out_offset=None, in_=wd_b[:, :],
                                                           in_offset=bass.IndirectOffsetOnAxis(ap=widx[:, s:s + 1], axis=0)),
                  reads=[Bpos, Bwcv], writes=[BW[s2]], dma=True, join=True)

        def slot_T(s):
            s2 = s % 2
            s3 = s % 3
            for i in range(2):
                transposes_bf16(xsl[s3][:, i, :], Bxsl[s3], i, xT[s2][:, :, i * 128:(i + 1) * 128], BxT[s2], evac=("scalar" if i else "vector"))

        def slot_body(s):
            s2 = s % 2
            if s + 2 < NS:
                slot_loads_x(s + 2)
            if s + 1 < NS:
                slot_loads(s + 1)
            for fc in range(4):
                gb = 2 + (fc % 2) * 2
                ub = gb + 1
                for kc in range(8):
                    P.add("tensor", lambda e, fc=fc, kc=kc, gb=gb: e.matmul(bk(gb)[:, 0:256], lhsT=Wgu[s2][:, kc, fc * 128:(fc + 1) * 128], rhs=xT[s2][:, kc, :], start=(kc == 0), stop=(kc == 7)),
                          reads=[BW[s2], BxT[s2]], writes=[bankB[gb]])
                for kc in range(8):
                    P.add("tensor", lambda e, fc=fc, kc=kc, ub=ub: e.matmul(bk(ub)[:, 0:256], lhsT=Wgu[s2][:, kc, 512 + fc * 128:512 + (fc + 1) * 128], rhs=xT[s2][:, kc, :], start=(kc == 0), stop=(kc == 7)),
                          reads=[BW[s2], BxT[s2]], writes=[bankB[ub]])
                f2 = fc % 2
                P.add("scalar", lambda e, gb=gb, f2=f2: e.activation(out=sg[f2], in_=bk(gb)[:, 0:256], func=AF.Silu), reads=[bankB[gb]], writes=[Bsg[f2]])
                P.add("vector", lambda e, fc=fc, ub=ub, f2=f2: e.tensor_tensor(out=hT[s2][:, fc, :], in0=sg[f2], in1=bk(ub)[:, 0:256], op=ALU.mult),
                      reads=[Bsg[f2], bankB[ub]], writes=[BhT[s2]])
            if s + 1 < NS:
                slot_T(s + 1)
            for i in range(2):
                for hf in range(2):
                    ob = 6 + hf
                    for fc in range(4):
                        P.add("tensor", lambda e, i=i, hf=hf, fc=fc, ob=ob: e.matmul(bk(ob), lhsT=hT[s2][:, fc, i * 128:(i + 1) * 128], rhs=Wd[s2][:, fc, hf * 512:(hf + 1) * 512],
                                                                                      start=(fc == 0), stop=(fc == 3)),
                              reads=[BhT[s2], BW[s2]], writes=[bankB[ob]])
                    if hf:
                        P.add("scalar", lambda e, i=i, hf=hf, ob=ob: e.activation(out=ys[s2][:, i, hf * 512:(hf + 1) * 512], in_=bk(ob), func=AF.Copy), reads=[bankB[ob]], writes=[Bys[s2]])
                    else:
                        P.add("vector", lambda e, i=i, hf=hf, ob=ob: e.tensor_copy(out=ys[s2][:, i, hf * 512:(hf + 1) * 512], in_=bk(ob)), reads=[bankB[ob]], writes=[Bys[s2]])
            P.add("sync", lambda e, s=s: e.dma_start(out=y_s[s * TSLOT:(s + 1) * TSLOT, :].rearrange("(i p) d -> p i d", p=128), in_=ys[s2]),
                  reads=[Bys[s2]], writes=[Bys_s], dma=True, store=True, join=(s > 0))

        slot_loads_x(0)
        slot_loads_x(1)
        slot_loads(0)
        slot_T(0)
        for s in range(NS):
            slot_body(s)
        P.barrier()

        M.off = slot_mark
        gfin = M.alloc([128, D], F32)
        Bgf = P.buf("gfin")
        P.add("sync", lambda e: e.dma_start(out=gfin, in_=norm_final[0:1, :].partition_broadcast(128)), writes=[Bgf], dma=True)
        NB4 = 4
        y1 = [M.alloc([128, D], BF16) for _ in range(NB4)]
        y2 = [M.alloc([128, D], BF16) for _ in range(NB4)]
        h2t = [M.alloc([128, D], F32) for _ in range(NB4)]
        acc = [M.alloc([128, D], F32) for _ in range(2)]
        ot = [M.alloc([128, D], F32) for _ in range(3)]
        ssF = [M.alloc([128, 4], F32) for _ in range(2)]
        By1 = [P.buf("y1_%d" % i) for i in range(NB4)]
        By2 = [P.buf("y2_%d" % i) for i in range(NB4)]
        Bh2t = [P.buf("h2t%d" % i) for i in range(NB4)]
        Bacc = [P.buf("acc%d" % i) for i in range(2)]
        Bot = [P.buf("ot%d" % i) for i in range(3)]
        BssF = [P.buf("ssF%d" % i) for i in range(2)]
        Bout = P.buf("out")
        junk = M.alloc([128, D], BF16)

        def fin_load(t):
            p4 = t % NB4
            P.add("sync", lambda e, t=t, p4=p4: e.dma_start(out=h2t[p4], in_=h2_s[t * 128:(t + 1) * 128, :]), reads=[Bh2s], writes=[Bh2t[p4]], dma=True)
            for k, (yy, By) in enumerate(((y1, By1), (y2, By2))):
                P.add("gpsimd", lambda e, t=t, p4=p4, k=k, yy=yy: e.indirect_dma_start(out=yy[p4], out_offset=None, in_=y_s[:, :],
                                                                                       in_offset=bass.IndirectOffsetOnAxis(ap=posi[:, t, k:k + 1], axis=0)),
                      reads=[Bys_s, Bpos], writes=[By[p4]], dma=True)

        def fin_tile(t):
            p2 = t % 2
            p3 = t % 3
            p4 = t % NB4
            P.add("vector", lambda e, t=t, p2=p2, p4=p4: e.scalar_tensor_tensor(out=acc[p2], in0=y1[p4], scalar=c12[:, t, 0:1], in1=h2t[p4], op0=ALU.mult, op1=ALU.add),
                  reads=[By1[p4], Bh2t[p4], BA], writes=[Bacc[p2]])
            P.add("vector", lambda e, t=t, p2=p2, p4=p4: e.scalar_tensor_tensor(out=acc[p2], in0=y2[p4], scalar=c12[:, t, 1:2], in1=acc[p2], op0=ALU.mult, op1=ALU.add),
                  reads=[By2[p4], Bacc[p2], BA], writes=[Bacc[p2]])
            rms_stats(acc[p2], D, ssF[p2], Bacc[p2], BssF[p2], junk)
            P.add("vector", lambda e, p2=p2, p3=p3: e.scalar_tensor_tensor(out=ot[p3], in0=acc[p2], scalar=ssF[p2][:, 2:3], in1=gfin, op0=ALU.mult, op1=ALU.mult),
                  reads=[Bacc[p2], BssF[p2], Bgf], writes=[Bot[p3]])

        def fin_store(t):
            p3 = t % 3
            P.add("sync", lambda e, t=t, p3=p3: e.dma_start(out=out[t * 128:(t + 1) * 128, :], in_=ot[p3]), reads=[Bot[p3]], writes=[Bout], dma=True, store=True, join=(t > 0))

        for t in range(min(NB4 - 1, NTILE)):
            fin_load(t)
        for t in range(NTILE):
            fin_tile(t)
            if t + NB4 - 1 < NTILE:
                fin_load(t + NB4 - 1)
            fin_store(t)
        if SCHED is not None:
            P.schedule(None if SCHED == "all" else SCHED)
        P.emit(st)
    return nc


def _consts():
    bf = ml_dtypes.bfloat16
    c = {}
    c["c_ident"] = np.eye(128, dtype=np.float32)
    c["c_identb"] = np.eye(128, dtype=np.float32).astype(bf)
    k = np.arange(128)
    c["c_tri"] = (k[None, :] >= k[:, None]).astype(np.float32).astype(bf)
    c["c_ltri"] = (k[:, None] < k[None, :]).astype(np.float32).astype(bf)
    pos = np.arange(SEQ)
    km = np.zeros((8, 12, SEQ), np.float32)
    qm = np.zeros((8, 4, SEQ), np.float32)
    for h in range(8):
        slope = 2.0 ** (-8.0 * (h + 1) / 8)
        for bl in range(8):
            km[h, bl, bl * 256:(bl + 1) * 256] = 1.0
        km[h, 8] = slope * 256.0 * (pos // 256)
        km[h, 9] = slope * (pos % 256)
        km[h, 10] = 1.0
        km[h, 11] = 1.0
        qm[h, 0] = 1.0
        qm[h, 1] = 1.0
        qm[h, 2] = -slope * 256.0 * (pos // 256)
        qm[h, 3] = -slope * (pos % 256)
    c["c_kmoba"] = km.astype(bf)
    c["c_qmoba"] = qm.astype(bf)
    kd = np.zeros((4, 4, SEQ), np.float32)
    qd = np.zeros((4, 4, SEQ), np.float32)
    for h in range(4):
        slope = 2.0 ** (-8.0 * (h + 1) / 4)
        kd[h, 0] = slope * 256.0 * (pos // 256)
        kd[h, 1] = slope * (pos % 256)
        kd[h, 2] = 1.0
        kd[h, 3] = 1.0
        qd[h, 0] = 1.0
        qd[h, 1] = 1.0
        qd[h, 2] = -slope * 256.0 * (pos // 256)
        qd[h, 3] = -slope * (pos % 256)
    c["c_kdiff"] = kd.astype(bf)
    c["c_qdiff"] = qd.astype(bf)
    past = np.zeros((2, 16, 8), np.float32)
    for t in range(16):
        own = t // 2
        for bl in range(8):
            past[0, t, bl] = 0.0 if bl < own else -1e30
            past[1, t, bl] = 1.0 if bl < own else 0.0
    c["c_past"] = past.reshape(2, 128)
    misc = np.zeros((128, 256), np.float32)
    misc[:, 0:32] = 256.0 * np.arange(32)[None, :]
    misc[:, 32:64] = np.arange(32)[None, :]
    misc[:, 64:64 + NS] = 256.0 * np.arange(NS)[None, :]
    p = np.arange(128)[:, None]
    misc[:, 160:168] = np.arange(8)[None, :] * 128 + p
    misc[:, 168:172] = np.arange(4)[None, :] * 128 + p
    c["c_misc"] = misc
    return c


_CACHE = {}


def kernel(x, mem, norm_mix, w_in, lambda_q1, lambda_k1, lambda_q2, lambda_k2, diff_subln,
           norm_moba_out, w_out, norm_mem_q, norm_mem_kv, w_mem_q, w_mem_kv, w_mem_o,
           norm_ffn, w_router_group, b_router_group, w_router_expert, b_router_expert,
           w_expert_gate, w_expert_up, w_expert_down, norm_final, _debug=False):
    f = lambda a: np.ascontiguousarray(np.asarray(a, dtype=np.float32))
    x = f(x)
    mem = f(mem)
    shared = dict(
        w_in=f(w_in)[0], w_out=f(w_out)[0], w_mem_q=f(w_mem_q)[0], w_mem_kv=f(w_mem_kv)[0], w_mem_o=f(w_mem_o)[0],
        w_rg=f(w_router_group)[0], w_re=f(w_router_expert)[0], b_rg=f(b_router_group).reshape(1, 4), b_re=f(b_router_expert).reshape(1, 32),
        w_gate=f(w_expert_gate)[0].reshape(32 * D, 512), w_up=f(w_expert_up)[0].reshape(32 * D, 512), w_down=f(w_expert_down)[0].reshape(32 * 512, D),
        norm_mix=f(norm_mix).reshape(1, D), norm_moba=f(norm_moba_out).reshape(1, 512), subln=f(diff_subln).reshape(1, 128),
        norm_mem_q=f(norm_mem_q).reshape(1, D), norm_mem_kv=f(norm_mem_kv).reshape(1, D), norm_ffn=f(norm_ffn).reshape(1, D),
        norm_final=f(norm_final).reshape(1, D),
        lam4=np.concatenate([f(lambda_q1).reshape(1, 64), f(lambda_k1).reshape(1, 64), f(lambda_q2).reshape(1, 64), f(lambda_k2).reshape(1, 64)], axis=0),
    )
    shared.update(_consts())
    key = bool(_debug)
    if key not in _CACHE:
        _CACHE[key] = build(debug=_debug)
    nc = _CACHE[key]
    in_maps = []
    for c in range(NCORES):
        m = dict(shared)
        m["x"] = x[4 * c:4 * c + 4].reshape(NTOK, D)
        m["mem"] = mem[4 * c:4 * c + 4].reshape(1024, D)
        in_maps.append(m)
    res = run_bass_kernel_spmd(nc, in_maps, core_ids=list(range(NCORES)))
    if _debug:
        return res
    outp = np.concatenate([np.asarray(r["out"]).reshape(4, SEQ, D) for r in res.results], axis=0)
    return outp.astype(np.float32)
```

```python
import math
from contextlib import ExitStack

import numpy as np
import ml_dtypes
import concourse.bass as bass
import concourse.mybir as mybir
from concourse.bass_utils import run_bass_kernel_spmd

F32 = mybir.dt.float32
BF16 = mybir.dt.bfloat16
I32 = mybir.dt.int32
AF = mybir.ActivationFunctionType
ALU = mybir.AluOpType
AX = mybir.AxisListType

ENGS = ("sync", "scalar", "gpsimd", "vector", "tensor")
EPOCH = 8000
NCORES = 8
SEQ = 2048
D = 1024
NTOK = 4 * SEQ
NTILE = NTOK // 128
TSLOT = 256
NS = 96
EPS = 1e-6
NEG = -30000.0
SBUF_BYTES = 207872
PE_SMALL_PEN = 40
P3_WARM = 0
P3_WARM_PEN = 400
P3_LAG = 0
P2_WARM = 256
SCHED = "all"


class Buf:
    def __init__(self, name):
        self.name = name
        self.last_w = None
        self.readers = []
        self.sem = None
        self.cnt = 0
        self.gen_deps = []
        self.ssem = None
        self.scnt = 0
        self.group = []


class Op:
    __slots__ = ("eng", "fn", "deps", "needed", "sem", "val", "dma", "wbuf", "alldeps", "idx", "phase", "fin", "busy", "lat", "pen", "store", "sbuf")

    def __init__(self, eng, fn, dma, wbuf):
        self.eng = eng
        self.fn = fn
        self.deps = []
        self.alldeps = []
        self.needed = False
        self.sem = None
        self.val = 0
        self.dma = dma
        self.wbuf = wbuf
        self.idx = 0
        self.phase = 0
        self.fin = None
        self.pen = 0
        self.store = False
        self.sbuf = None


class _Probe:
    def __init__(self):
        self.rec = None

    def __getattr__(self, name):
        def f(*a, **kw):
            self.rec = (name, a, kw)
            return self
        return f


def _est(op):
    pr = _Probe()
    op.fn(pr)
    name, a, kw = pr.rec
    out = kw.get("out", a[0] if a else None)

    def fs(ap):
        sh = ap.shape
        n_ = 1
        for v in sh[1:]:
            n_ *= int(v)
        return n_
    n = fs(out)
    for k in ("in_", "in0"):
        if k in kw and kw[k] is not None:
            try:
                n = max(n, fs(kw[k]))
            except Exception:
                pass
    if name in ("matmul", "transpose"):
        t = fs(out) / 2400.0 + 0.02
        return t, t + 0.16
    esz = 2 if out.dtype == BF16 else 4
    if name == "dma_start":
        nb = fs(out) * int(out.shape[0]) * esz
        return (0.6 if op.eng == "gpsimd" else 0.06), 2.0 + nb / 220e3
    if name == "indirect_dma_start":
        nb = min(fs(out) * int(out.shape[0]), fs(kw["in_"]) * int(kw["in_"].shape[0])) * esz
        return 1.0, 3.0 + nb / 220e3
    if op.eng == "scalar":
        t = n * 0.00076 + 0.22
    elif op.eng == "vector":
        t = n * 0.00095 + 0.22
    else:
        t = n * 0.0021 + 0.3
    return t, t


class Prog:
    def __init__(self, nc):
        self.nc = nc
        self.streams = {e: [] for e in ENGS}
        self.sems = []
        self.all_ops = []
        self.bufs = []
        self.pending = {e: [] for e in ENGS}
        self.phase = 0
        self.phase_evs = {}

    def buf(self, name):
        b = Buf(name)
        self.bufs.append(b)
        return b

    def add(self, eng, fn, reads=(), writes=(), dma=False, join=False, after=(), store=False):
        op = Op(eng, fn, dma, writes[0] if (dma and writes) else None)
        op.store = bool(store and dma)
        if dma:
            op.sbuf = reads[0] if op.store else writes[0]
        deps = list(self.pending[eng])
        self.pending[eng] = []
        deps.extend(after)
        for b in reads:
            deps.extend(b.group)
        for b in writes:
            if join and dma:
                deps.extend(b.gen_deps)
                deps.extend(b.readers)
            else:
                g = list(b.readers)
                g.extend(b.group)
                b.gen_deps = g
                deps.extend(g)
        seen = set()
        for d in deps:
            if d is op or id(d) in seen:
                continue
            seen.add(id(d))
            op.alldeps.append(d)
            if d.eng == "tensor" and eng == "tensor" and not d.dma and not dma:
                continue
            op.deps.append(d)
            d.needed = True
        if dma and join and writes and writes[0].last_w is not None and id(writes[0].last_w) not in seen:
            op.alldeps.append(writes[0].last_w)
        op.idx = len(self.all_ops)
        op.phase = self.phase
        for b in reads:
            b.readers.append(op)
        for b in writes:
            if join and dma:
                b.group.append(op)
            else:
                b.group = [op]
            b.last_w = op
            b.readers = []
        self.streams[eng].append(op)
        self.all_ops.append(op)
        return op

    def barrier(self):
        evs = []
        for b in self.bufs:
            if getattr(b, "nobar", False):
                continue
            evs.extend(b.group)
            evs.extend(b.readers)
        for e in ENGS:
            if self.streams[e]:
                last = [o for o in self.streams[e][-4:] if not o.dma]
                evs.extend(last[-1:])
        for e in ENGS:
            self.pending[e] = list(evs)
        self.phase += 1
        self.phase_evs[self.phase] = list(evs)

    def schedule(self, phases=None):
        import heapq
        succ = {}

        def prio(o):
            if PE_SMALL_PEN and o.eng == "tensor" and o.busy < 0.07:
                return o.idx + PE_SMALL_PEN
            return o.idx + o.pen
        for op in self.all_ops:
            op.busy, op.lat = _est(op)
        for op in self.all_ops:
            for d in op.alldeps:
                succ.setdefault(id(d), []).append(op)
        free_at = {e: 0.0 for e in ENGS}
        new_streams = {e: [] for e in ENGS}
        order = []
        tmax = 0.0
        nph = self.phase + 1
        by_phase = [[] for _ in range(nph)]
        for op in self.all_ops:
            by_phase[op.phase].append(op)
        for p in range(nph):
            ops = by_phase[p]
            if phases is not None and p not in phases:
                t = max([tmax] + list(free_at.values()))
                for op in ops:
                    op.fin = t
                    new_streams[op.eng].append(op)
                    order.append(op)
                continue
            t0 = tmax
            indeg = {}
            rdy = {}
            pend = {e: [] for e in ENGS}
            avail = {e: [] for e in ENGS}
            for op in ops:
                c = 0
                r = t0
                for d in op.alldeps:
                    if d.fin is None:
                        c += 1
                    else:
                        r = max(r, d.fin + (0.0 if d.eng == op.eng and not d.dma else 0.15))
                indeg[id(op)] = c
                rdy[id(op)] = r
                if c == 0:
                    heapq.heappush(pend[op.eng], (r, prio(op), op.idx, op))
            for e in ENGS:
                free_at[e] = max(free_at[e], t0)
            left = len(ops)
            while left:
                best = None
                for e in ENGS:
                    T = free_at[e]
                    while pend[e] and pend[e][0][0] <= T:
                        r, i, i2, o = heapq.heappop(pend[e])
                        heapq.heappush(avail[e], (i, i2, o))
                    if avail[e]:
                        cand = (T, e, True)
                    elif pend[e]:
                        cand = (pend[e][0][0], e, False)
                    else:
                        continue
                    if best is None or cand[0] < best[0]:
                        best = cand
                assert best is not None, "scheduler deadlock"
                st_, e, from_avail = best
                if from_avail:
                    i, i2, op = heapq.heappop(avail[e])
                else:
                    r, i, i2, op = heapq.heappop(pend[e])
                free_at[e] = st_ + op.busy
                op.fin = st_ + op.lat
                tmax = max(tmax, op.fin)
                new_streams[e].append(op)
                order.append(op)
                left -= 1
                for q in succ.get(id(op), ()):
                    k = id(q)
                    if k not in indeg:
                        continue
                    rdy[k] = max(rdy[k], op.fin + (0.0 if q.eng == e and not op.dma else 0.15))
                    indeg[k] -= 1
                    if indeg[k] == 0:
                        heapq.heappush(pend[q.eng], (rdy[k], prio(q), q.idx, q))
        for e in ENGS:
            seen_ph = set()
            for op in new_streams[e]:
                if op.phase in seen_ph:
                    continue
                seen_ph.add(op.phase)
                evs = self.phase_evs.get(op.phase)
                if not evs:
                    continue
                have = set(id(d) for d in op.deps)
                for d in evs:
                    if d is op or id(d) in have:
                        continue
                    have.add(id(d))
                    op.deps.append(d)
                    d.needed = True
        self.streams = new_streams
        self.all_ops = order
        self.est_total = tmax

    def emit(self, stack):
        nc = self.nc
        cnts = {e: 0 for e in ENGS}
        curs = {e: None for e in ENGS}
        for op in self.all_ops:
            eng = op.eng
            if op.dma and op.store:
                b = op.sbuf
                if b.ssem is None:
                    b.ssem = stack.enter_context(nc.semaphore("s_" + b.name))
                    self.sems.append(b.ssem)
                b.scnt += 1
                op.sem = b.ssem
                op.val = 16 * b.scnt
            elif op.dma:
                b = op.sbuf
                if b.sem is None:
                    b.sem = stack.enter_context(nc.semaphore("d_" + b.name))
                    self.sems.append(b.sem)
                b.cnt += 1
                op.sem = b.sem
                op.val = 16 * b.cnt
            elif op.needed:
                if curs[eng] is None or cnts[eng] >= EPOCH:
                    curs[eng] = stack.enter_context(nc.semaphore("c_%s_%d" % (eng, len(self.sems))))
                    self.sems.append(curs[eng])
                    cnts[eng] = 0
                cnts[eng] += 1
                op.sem = curs[eng]
                op.val = cnts[eng]
        block = stack.enter_context(nc.Block())
        prog = self

        def run(e, eng):
            waited = {}
            for op in prog.streams[eng]:
                need = {}
                for d in op.deps:
                    k = id(d.sem)
                    if d.val > need.get(k, (None, 0))[1]:
                        need[k] = (d.sem, d.val)
                for k, (sem_, val_) in need.items():
                    if waited.get(k, 0) < val_:
                        e.wait_ge(sem_, val_)
                        waited[k] = val_
                ins = op.fn(e)
                if op.dma:
                    ins.then_inc(op.sem, 16)
                elif op.needed:
                    ins.then_inc(op.sem, 1)
            last = {}
            for op in prog.streams[eng]:
                if op.dma:
                    last[id(op.sem)] = (op.sem, max(op.val, last.get(id(op.sem), (None, 0))[1]))
            for s, v in last.values():
                if waited.get(id(s), 0) < v:
                    e.wait_ge(s, v)

        @block.sync
        def _(e):
            run(e, "sync")

        @block.scalar
        def _(e):
            run(e, "scalar")

        @block.gpsimd
        def _(e):
            run(e, "gpsimd")

        @block.vector
        def _(e):
            run(e, "vector")

        @block.tensor
        def _(e):
            run(e, "tensor")


class Mem:
    def __init__(self, big):
        self.big = big
        self.off = 0

    def alloc(self, shape, dt=BF16):
        esz = 2 if dt == BF16 else 4
        n = int(np.prod(shape[1:])) * esz
        off = self.off
        self.off += (n + 63) // 64 * 64
        assert self.off <= SBUF_BYTES, ("SBUF overflow", self.off)
        ap = self.big[0:shape[0], off // 2:(off + n) // 2]
        if dt != BF16:
            ap = ap.bitcast(dt)
        if len(shape) > 2:
            names = ["d%d" % i for i in range(len(shape) - 1)]
            kw = {nm: int(s) for nm, s in zip(names[1:], shape[2:])}
            ap = ap.rearrange("p (%s) -> p %s" % (" ".join(names), " ".join(names)), **kw)
        return ap


def bc(ap, shape, axis):
    return ap.unsqueeze(axis).to_broadcast(list(shape))


def build(debug=False):
    nc = bass.Bass("TRN2", target_bir_lowering=False)

    def din(name, shape, dt=F32):
        return nc.dram_tensor(name, list(shape), dt, kind="ExternalInput").ap()

    def dscr(name, shape, dt):
        return nc.dram_tensor(name, list(shape), dt, kind=("ExternalOutput" if debug else "Internal")).ap()

    x = din("x", [NTOK, D])
    mem = din("mem", [1024, D])
    w_in = din("w_in", [D, 3072])
    w_out = din("w_out", [D, D])
    w_mem_q = din("w_mem_q", [D, D])
    w_mem_kv = din("w_mem_kv", [D, 2048])
    w_mem_o = din("w_mem_o", [D, D])
    w_rg = din("w_rg", [D, 4])
    w_re = din("w_re", [D, 32])
    b_rg = din("b_rg", [1, 4])
    b_re = din("b_re", [1, 32])
    w_gate = din("w_gate", [32 * D, 512])
    w_up = din("w_up", [32 * D, 512])
    w_down = din("w_down", [32 * 512, D])
    norm_mix = din("norm_mix", [1, D])
    norm_moba = din("norm_moba", [1, 512])
    subln = din("subln", [1, 128])
    norm_mem_q = din("norm_mem_q", [1, D])
    norm_mem_kv = din("norm_mem_kv", [1, D])
    norm_ffn = din("norm_ffn", [1, D])
    norm_final = din("norm_final", [1, D])
    lam4 = din("lam4", [4, 64])
    c_ident = din("c_ident", [128, 128])
    c_identb = din("c_identb", [128, 128], BF16)
    c_tri = din("c_tri", [128, 128], BF16)
    c_ltri = din("c_ltri", [128, 128], BF16)
    c_kmoba = din("c_kmoba", [8, 12, SEQ], BF16)
    c_qmoba = din("c_qmoba", [8, 4, SEQ], BF16)
    c_kdiff = din("c_kdiff", [4, 4, SEQ], BF16)
    c_qdiff = din("c_qdiff", [4, 4, SEQ], BF16)
    c_past = din("c_past", [2, 128])
    c_misc = din("c_misc", [128, 256])
    out = nc.dram_tensor("out", [NTOK, D], F32, kind="ExternalOutput").ap()

    qk_s = dscr("qk_s", [4, 64, 32, SEQ], BF16)
    v_s = dscr("v_s", [4, SEQ, 1040], BF16)
    att_s = dscr("att_s", [NTOK, D], F32)
    h2_s = dscr("h2_s", [NTOK, D], F32)
    xn_s = dscr("xn_s", [NTOK, D], BF16)
    xs_s = dscr("xs_s", [NS * TSLOT, D], BF16)
    y_s = dscr("y_s", [NS * TSLOT, D], BF16)
    dbg_s = dscr("dbg_s", [128, 1024], F32)
    wgu_b = dscr("wgu_b", [32 * 128, 8 * 1024], BF16)
    wd_b = dscr("wd_b", [32 * 128, 4 * 1024], BF16)

    st = ExitStack()
    with st:
        big = st.enter_context(nc.sbuf_tensor("big", [128, SBUF_BYTES // 2], BF16))
        banks = [st.enter_context(nc.psum_tensor("bank%d" % i, [128, 512], F32)) for i in range(8)]
        P = Prog(nc)
        M = Mem(big)
        bankB = [P.buf("bank%d" % i) for i in range(8)]

        def bk(i):
            return banks[i][:]

        def bkb(i):
            return banks[i][:].bitcast(BF16)

        ident_f = M.alloc([128, 128], F32)
        ident_b = M.alloc([128, 128], BF16)
        tri = M.alloc([128, 128], BF16)
        ltri = M.alloc([128, 128], BF16)
        ones_b = M.alloc([128, 128], BF16)
        misc = M.alloc([128, 256], F32)
        past = M.alloc([128, 2, 128], F32)
        eps_t = M.alloc([128, 1], F32)
        lamt = M.alloc([128, 4, 64], F32)
        lams = M.alloc([128, 8], F32)
        A1 = M.alloc([128, NTILE, 32], BF16)
        A2 = M.alloc([128, NTILE, 32], BF16)
        c12 = M.alloc([128, NTILE, 2], F32)
        posi = M.alloc([128, NTILE, 2], I32)
        widx = M.alloc([128, NS], I32)
        Bc = P.buf("consts")
        Blam = P.buf("lam")
        BA = P.buf("Atab")
        Bpos = P.buf("postab")
        first = [True]

        def cload(dst, src, eng="sync"):
            P.add(eng, lambda e: e.dma_start(out=dst, in_=src), writes=[Bc], dma=True, join=not first[0])
            first[0] = False

        cload(ident_f, c_ident[:, :])
        cload(ident_b, c_identb[:, :])
        cload(tri, c_tri[:, :])
        cload(ltri, c_ltri[:, :])
        cload(misc, c_misc[:, :])
        cload(past[:, 0, :], c_past[0:1, :].partition_broadcast(128))
        cload(past[:, 1, :], c_past[1:2, :].partition_broadcast(128))
        for i in range(4):
            cload(lamt[:, i, :], lam4[i:i + 1, :].partition_broadcast(128))
        P.add("vector", lambda e: e.memset(ones_b, 1.0), writes=[Bc])
        P.add("vector", lambda e: e.memset(eps_t, EPS), writes=[Bc])
        lam_init = 0.8 - 0.6 * math.exp(-0.3 * 0)
        ljunk = M.alloc([128, 64], F32)
        for i in range(2):
            P.add("vector", lambda e, i=i: e.tensor_tensor(out=ljunk, in0=lamt[:, 2 * i, :], in1=lamt[:, 2 * i + 1, :], op=ALU.mult),
                  reads=[Bc], writes=[Blam])
            P.add("vector", lambda e, i=i: e.tensor_reduce(out=lams[:, i:i + 1], in_=ljunk, axis=AX.X, op=ALU.add),
                  reads=[Blam], writes=[Blam])
        P.add("scalar", lambda e: e.activation(out=lams[:, 2:4], in_=lams[:, 0:2], func=AF.Exp), reads=[Blam], writes=[Blam])
        P.add("vector", lambda e: e.tensor_tensor(out=lams[:, 4:5], in0=lams[:, 3:4], in1=lams[:, 2:3], op=ALU.subtract),
              reads=[Blam], writes=[Blam])
        P.add("vector", lambda e: e.tensor_scalar(out=lams[:, 4:5], in0=lams[:, 4:5], scalar1=-lam_init, scalar2=None, op0=ALU.add),
              reads=[Blam], writes=[Blam])
        neglam = lams[:, 4:5]
        persist_mark = M.off

        JB = {}

        def jbuf(j):
            k = int(j.offset)
            if k not in JB:
                JB[k] = P.buf("junk%d" % len(JB))
            return JB[k]

        def rms_stats(src, width, ssb, Bsrc, Bss, junk):
            P.add("scalar", lambda e: e.activation(out=junk, in_=src, func=AF.Square, accum_out=ssb[:, 0:1]),
                  reads=[Bsrc], writes=[Bss, jbuf(junk)])
            P.add("scalar", lambda e: e.activation(out=ssb[:, 1:2], in_=ssb[:, 0:1], func=AF.Sqrt, bias=eps_t[:, 0:1], scale=1.0 / width),
                  reads=[Bss, Bc], writes=[Bss])
            P.add("vector", lambda e: e.reciprocal(out=ssb[:, 2:3], in_=ssb[:, 1:2]), reads=[Bss], writes=[Bss])

        def load_w_bf16(dst, src, ncols, B):
            k = 0
            for kc in range(8):
                for c0 in range(0, ncols, 1024):
                    P.add("gpsimd", lambda e, kc=kc, c0=c0: e.dma_start(out=dst[:, kc, c0:c0 + 1024], in_=src[kc * 128:(kc + 1) * 128, c0:c0 + 1024]),
                          writes=[B], dma=True, join=(k > 0))
                    k += 1

        def transposes_bf16(src, Bsrc, bank, dst, Bdst, n=8, evac="scalar"):
            pv = bkb(bank)
            for k in range(n):
                P.add("tensor", lambda e, k=k: e.transpose(pv[:, k * 128:(k + 1) * 128], src[:, k * 128:(k + 1) * 128], ident_b),
                      reads=[Bsrc, Bc], writes=[bankB[bank]])
            src_v = pv[:, 0:n * 128].rearrange("p (k t) -> p k t", k=n)
            if evac == "scalar":
                P.add("scalar", lambda e: e.activation(out=dst, in_=src_v, func=AF.Copy), reads=[bankB[bank]], writes=[Bdst])
            else:
                P.add("vector", lambda e: e.tensor_copy(out=dst, in_=src_v), reads=[bankB[bank]], writes=[Bdst])

        w_in_sb = M.alloc([128, 8, 3072], BF16)
        Bwin = P.buf("w_in")
        load_w_bf16(w_in_sb, w_in, 3072, Bwin)
        gmix = M.alloc([128, D], F32)
        Bg = P.buf("gmix")
        P.add("sync", lambda e: e.dma_start(out=gmix, in_=norm_mix[0:1, :].partition_broadcast(128)), writes=[Bg], dma=True)
        xt = [M.alloc([128, D], F32) for _ in range(2)]
        Bxt = [P.buf("xt%d" % i) for i in range(2)]
        ss1 = [M.alloc([128, 4], F32) for _ in range(2)]
        Bss1 = [P.buf("ss1_%d" % i) for i in range(2)]
        junk = M.alloc([128, D], BF16)
        hn = [M.alloc([128, D], BF16) for _ in range(2)]
        Bhn = [P.buf("hn%d" % i) for i in range(2)]
        hnT = [M.alloc([128, 8, 512], BF16) for _ in range(2)]
        BhnT = [P.buf("hnT%d" % i) for i in range(2)]
        qk_st = [M.alloc([128, 16, 512], BF16) for _ in range(2)]
        Bqkst = [P.buf("qkst%d" % i) for i in range(2)]
        vst = [M.alloc([128, 4, 1040], BF16) for _ in range(2)]
        Bvst = [P.buf("vst%d" % i) for i in range(2)]
        Bqk_s = [P.buf("qk_s%d" % b) for b in range(4)]
        for i in range(2):
            P.add("vector", lambda e, i=i: e.memset(vst[i], 1.0), writes=[Bvst[i]])

        Bwcv = P.buf("wconv")
        Bwcv.nobar = True
        wgu_v = wgu_b.rearrange("(e p) (kc two f) -> e p kc two f", p=128, kc=8, two=2)
        wd_v = wd_b.rearrange("(e p) (fc d) -> e p fc d", p=128, fc=4)
        ncv = [0]

        def conv_expert(E, after):
            for (two, src) in ((0, w_gate), (1, w_up)):
                P.add("gpsimd", lambda e, E=E, two=two, src=src: e.dma_start(out=wgu_v[E, :, :, two, :], in_=src[E * 1024:(E + 1) * 1024, :].rearrange("(kc p) f -> p kc f", p=128)),
                      writes=[Bwcv], dma=True, join=(ncv[0] > 0), after=after)
                ncv[0] += 1
            P.add("gpsimd", lambda e, E=E: e.dma_start(out=wd_v[E], in_=w_down[E * 512:(E + 1) * 512, :].rearrange("(fc p) d -> p fc d", p=128)),
                  writes=[Bwcv], dma=True, join=True, after=after)
            ncv[0] += 1

        NCV1 = 10

        pcols = [k * 128 for k in range(4)] + [512 + k * 128 for k in range(4)] + [1536 + k * 128 for k in range(4)] + [2048 + k * 128 for k in range(4)]
        pscale = [0.125] * 4 + [1.0] * 4 + [0.125] * 4 + [1.0] * 4
        evq = [0]
        lastmm = [None]

        def p1_norm(ck, i):
            s2 = ck % 2
            t = ck * 4 + i
            p2 = t % 2
            P.add("sync", lambda e, t=t, p2=p2: e.dma_start(out=xt[p2], in_=x[t * 128:(t + 1) * 128, :]), writes=[Bxt[p2]], dma=True)
            rms_stats(xt[p2], D, ss1[p2], Bxt[p2], Bss1[p2], junk)
            P.add("vector", lambda e, p2=p2: e.scalar_tensor_tensor(out=hn[p2], in0=xt[p2], scalar=ss1[p2][:, 2:3], in1=gmix, op0=ALU.mult, op1=ALU.mult),
                  reads=[Bxt[p2], Bss1[p2], Bg], writes=[Bhn[p2]])
            transposes_bf16(hn[p2], Bhn[p2], p2, hnT[s2][:, :, i * 128:(i + 1) * 128], BhnT[s2], evac=("scalar" if i % 2 else "vector"))

        def p1_mm(ck):
            b, c = ck // 4, ck % 4
            s2 = ck % 2
            for g in range(16):
                bnk = 2 + g % 4
                for kc in range(8):
                    P.add("tensor", lambda e, g=g, kc=kc, bnk=bnk, s2=s2: e.matmul(bk(bnk), lhsT=w_in_sb[:, kc, pcols[g]:pcols[g] + 128], rhs=hnT[s2][:, kc, :],
                                                                                 start=(kc == 0), stop=(kc == 7)),
                          reads=[Bwin, BhnT[s2]], writes=[bankB[bnk]])
                if evq[0] % 2 == 0:
                    P.add("scalar", lambda e, g=g, bnk=bnk, s2=s2: e.activation(out=qk_st[s2][:, g, :], in_=bk(bnk), func=AF.Copy, scale=pscale[g]),
                          reads=[bankB[bnk]], writes=[Bqkst[s2]])
                else:
                    P.add("vector", lambda e, g=g, bnk=bnk, s2=s2: e.tensor_scalar(out=qk_st[s2][:, g, :], in0=bk(bnk), scalar1=pscale[g], scalar2=None, op0=ALU.mult),
                          reads=[bankB[bnk]], writes=[Bqkst[s2]])
                evq[0] += 1
                if g % 4 == 1 and ck + 1 < 16:
                    p1_norm(ck + 1, g // 4)
            qv = qk_s[b].rearrange("d (k two) s -> d k two s", two=2)
            for hh in range(2):
                P.add("sync", lambda e, c=c, s2=s2, hh=hh, qv=qv: e.dma_start(out=qv[:, :, hh, c * 512:(c + 1) * 512], in_=qk_st[s2][hh * 64:(hh + 1) * 64, :, :]),
                      reads=[Bqkst[s2]], writes=[Bqk_s[b]], dma=True, store=True, join=(c > 0 or hh > 0))
            for i in range(4):
                for hf in range(2):
                    bnk = 6 + hf
                    vc = 1024 if hf == 0 else 2560
                    for kc in range(8):
                        lastmm[0] = P.add("tensor", lambda e, kc=kc, i=i, bnk=bnk, vc=vc, s2=s2: e.matmul(bk(bnk), lhsT=hnT[s2][:, kc, i * 128:(i + 1) * 128], rhs=w_in_sb[:, kc, vc:vc + 512],
                                                                                            start=(kc == 0), stop=(kc == 7)),
                              reads=[Bwin, BhnT[s2]], writes=[bankB[bnk]])
                    dstv = vst[s2][:, i, hf * 520:(hf + 1) * 520].rearrange("p (h c) -> p h c", c=65)[:, :, 0:64]
                    srcv = bk(bnk).rearrange("p (h c) -> p h c", c=64)
                    if hf == 0:
                        P.add("scalar", lambda e, dstv=dstv, srcv=srcv: e.activation(out=dstv, in_=srcv, func=AF.Copy), reads=[bankB[bnk]], writes=[Bvst[s2]])
                    else:
                        P.add("vector", lambda e, dstv=dstv, srcv=srcv: e.tensor_copy(out=dstv, in_=srcv), reads=[bankB[bnk]], writes=[Bvst[s2]])
            P.add("sync", lambda e, b=b, c=c, s2=s2: e.dma_start(out=v_s[b, c * 512:(c + 1) * 512, :].rearrange("(i p) d -> p i d", p=128), in_=vst[s2]),
                  reads=[Bvst[s2]], writes=[Bqk_s[b]], dma=True, store=True, join=True)
            for E in range(NCV1):
                if (E * 16) // NCV1 == ck:
                    conv_expert(E, [lastmm[0]])

        for i in range(4):
            p1_norm(0, i)
        for ck in range(16):
            p1_mm(ck)
        P.barrier()

        M.off = persist_mark
        VS = [M.alloc([128, 16, 1040], BF16) for _ in range(2)]
        BVS = [P.buf("VS%d" % i) for i in range(2)]
        KAm = [M.alloc([76, SEQ], BF16) for _ in range(2)]
        QAm = [M.alloc([76, SEQ], BF16) for _ in range(2)]
        KAd = [M.alloc([68, SEQ], BF16) for _ in range(2)]
        QAd = [M.alloc([68, SEQ], BF16) for _ in range(2)]
        BKAm = [P.buf("KAm%d" % i) for i in range(2)]
        BQAm = [P.buf("QAm%d" % i) for i in range(2)]
        BKAd = [P.buf("KAd%d" % i) for i in range(2)]
        BQAd = [P.buf("QAd%d" % i) for i in range(2)]
        NPT = 6
        PT = [M.alloc([128, 512], BF16) for _ in range(NPT)]
        BPT = [P.buf("PT%d" % i) for i in range(NPT)]
        OT = [M.alloc([65, 512], F32) for _ in range(2)]
        BOT = [P.buf("OT%d" % i) for i in range(2)]
        ATT = M.alloc([128, 16, D], F32)
        BATT = [P.buf("ATT%d" % c) for c in range(4)]
        Batt_s = [P.buf("att_s%d" % b) for b in range(4)]
        gt = [M.alloc([128, 16, 8], F32) for _ in range(4)]
        gm = M.alloc([128, 3, 16], F32)
        Bgt = P.buf("gate_tmp")
        MB = [M.alloc([128, 16, 72], BF16) for _ in range(2)]
        BMB = [P.buf("MB%d" % i) for i in range(2)]
        km = M.alloc([64, 8], F32)
        kmb = [M.alloc([64, 8], BF16) for _ in range(2)]
        Bkm = P.buf("km")
        Bkmb = [P.buf("kmb%d" % i) for i in range(2)]
        rc = M.alloc([128, 8], F32)
        Brc = P.buf("rc")
        ftmp = M.alloc([128, 4, 64], F32)
        Bftmp = P.buf("ftmp")
        for i in range(2):
            P.add("vector", lambda e, i=i: e.memset(MB[i], 0.0), writes=[BMB[i]])
        SB_ = [0, 1, 2]
        MISC = 7

        units = []
        for b in range(4):
            for uu in range(16):
                units.append((b, uu))

        def unit_info(u):
            b, uu = units[u]
            moba = uu < 8
            slot = (uu % 2) if moba else ((uu - 8) % 2)
            return b, uu, moba, slot

        def pro_loads(u):
            b, uu, moba, slot = unit_info(u)
            if uu == 0:
                P.add("sync", lambda e: e.dma_start(out=VS[b % 2], in_=v_s[b, :, :].rearrange("(j p) d -> p j d", p=128)),
                      reads=[Bqk_s[b]], writes=[BVS[b % 2]], dma=True)
            if moba:
                h = uu
                P.add("sync", lambda e: e.dma_start(out=KAm[slot][0:64, :], in_=qk_s[b, :, 8 + h, :]), reads=[Bqk_s[b]], writes=[BKAm[slot]], dma=True)
                P.add("sync", lambda e: e.dma_start(out=KAm[slot][64:76, :], in_=c_kmoba[h, :, :]), writes=[BKAm[slot]], dma=True, join=True)
                P.add("sync", lambda e: e.dma_start(out=QAm[slot][0:64, :], in_=qk_s[b, :, h, :]), reads=[Bqk_s[b]], writes=[BQAm[slot]], dma=True)
                P.add("sync", lambda e: e.dma_start(out=QAm[slot][72:76, :], in_=c_qmoba[h, :, :]), writes=[BQAm[slot]], dma=True, join=True)
            else:
                j = uu - 8
                h = j // 2
                P.add("sync", lambda e: e.dma_start(out=KAd[slot][0:64, :], in_=qk_s[b, :, 24 + j, :]), reads=[Bqk_s[b]], writes=[BKAd[slot]], dma=True)
                P.add("sync", lambda e: e.dma_start(out=KAd[slot][64:68, :], in_=c_kdiff[h, :, :]), writes=[BKAd[slot]], dma=True, join=True)
                P.add("sync", lambda e: e.dma_start(out=QAd[slot][0:64, :], in_=qk_s[b, :, 16 + j, :]), reads=[Bqk_s[b]], writes=[BQAd[slot]], dma=True)
                P.add("sync", lambda e: e.dma_start(out=QAd[slot][64:68, :], in_=c_qdiff[h, :, :]), writes=[BQAd[slot]], dma=True, join=True)

        def pro_compute(u, part):
            b, uu, moba, slot = unit_info(u)
            if not moba:
                return
            g1, g2, ge, g3 = gt
            if part == 0:
                P.add("vector", lambda e: e.tensor_reduce(out=km, in_=KAm[slot][0:64, :].rearrange("p (n l) -> p n l", l=256), axis=AX.X, op=ALU.add),
                      reads=[BKAm[slot]], writes=[Bkm])
                P.add("vector", lambda e: e.tensor_copy(out=kmb[slot], in_=km), reads=[Bkm], writes=[Bkmb[slot]])
                return
            if part == 2:
                for q4 in range(4):
                    for tt in range(4):
                        t = q4 * 4 + tt
                        P.add("tensor", lambda e, t=t, tt=tt: e.matmul(bk(MISC)[0:72, tt * 128:(tt + 1) * 128], lhsT=MB[slot][:, t, :], rhs=ident_b, start=True, stop=True),
                              reads=[BMB[slot], Bc], writes=[bankB[MISC]])
                    P.add("vector", lambda e, q4=q4: e.tensor_copy(out=QAm[slot][64:72, q4 * 512:(q4 + 1) * 512], in_=bk(MISC)[64:72, :]),
                          reads=[bankB[MISC]], writes=[BQAm[slot]])
                return
            for t in range(16):
                P.add("tensor", lambda e, t=t: e.matmul(bk(MISC)[:, t * 8:(t + 1) * 8], lhsT=QAm[slot][0:64, t * 128:(t + 1) * 128], rhs=kmb[slot],
                                                        start=True, stop=True),
                      reads=[BQAm[slot], Bkmb[slot]], writes=[bankB[MISC]])
            gps = bk(MISC)[:, 0:128].rearrange("p (t n) -> p t n", n=8)
            pb = past[:, 0, :].rearrange("p (t n) -> p t n", n=8)
            psel = past[:, 1, :].rearrange("p (t n) -> p t n", n=8)
            P.add("vector", lambda e: e.tensor_tensor(out=g1, in0=gps, in1=pb, op=ALU.add), reads=[bankB[MISC], Bc], writes=[Bgt])
            cur = g1
            for r in range(3):
                P.add("vector", lambda e, cur=cur, r=r: e.tensor_reduce(out=gm[:, r, :], in_=cur, axis=AX.X, op=ALU.max), reads=[Bgt], writes=[Bgt])
                if r < 2:
                    nxt = g2 if r == 0 else g3
                    P.add("vector", lambda e, cur=cur, r=r: e.tensor_tensor(out=ge, in0=cur, in1=bc(gm[:, r, :], [128, 16, 8], 2), op=ALU.is_ge),
                          reads=[Bgt], writes=[Bgt])
                    P.add("vector", lambda e, cur=cur, nxt=nxt: e.scalar_tensor_tensor(out=nxt, in0=ge, scalar=-1e30, in1=cur, op0=ALU.mult, op1=ALU.add),
                          reads=[Bgt], writes=[Bgt])
                    cur = nxt
            P.add("vector", lambda e: e.tensor_tensor(out=ge, in0=g1, in1=bc(gm[:, 2, :], [128, 16, 8], 2), op=ALU.is_lt), reads=[Bgt], writes=[Bgt])
            P.add("vector", lambda e: e.scalar_tensor_tensor(out=MB[slot][:, :, 64:72], in0=ge, scalar=NEG, in1=psel, op0=ALU.mult, op1=ALU.mult),
                  reads=[Bgt, Bc], writes=[BMB[slot]])

        tasks = []
        for u in range(len(units)):
            for c in range(4):
                nj = 4 * c + 4
                for j in range(nj):
                    tasks.append(dict(u=u, c=c, j=j, o=(j - 4 * c if j >= 4 * c else None), first=(j == 0), last=(j == nj - 1),
                                      ufirst=(c == 0 and j == 0), cfirst=(c if j == 0 else None)))
        chunk_ctr = [0]

        def opair(tk):
            return tk["pair"]

        def issue_S(ti):
            tk = tasks[ti]
            u = tk["u"]
            b, uu, moba, slot = unit_info(u)
            if tk["ufirst"]:
                if u == 0:
                    pro_loads(0)
                    for part in range(3):
                        pro_compute(0, part)
                if u + 1 < len(units):
                    pro_loads(u + 1)
            if tk["cfirst"] in (1, 2, 3) and u + 1 < len(units):
                pro_compute(u + 1, tk["cfirst"] - 1)
            KA, QA, BK, BQ, nr = (KAm[slot], QAm[slot], BKAm[slot], BQAm[slot], 76) if moba else (KAd[slot], QAd[slot], BKAd[slot], BQAd[slot], 68)
            sb = SB_[ti % 3]
            c, j = tk["c"], tk["j"]
            lo = 0 if tk["o"] is None else tk["o"] * 128
            tk["sb"] = sb
            tk["lo"] = lo
            sop = P.add("tensor", lambda e: e.matmul(bk(sb)[:, lo:512], lhsT=KA[0:nr, j * 128:(j + 1) * 128], rhs=QA[0:nr, c * 512 + lo:(c + 1) * 512], start=True, stop=True),
                        reads=[BK, BQ], writes=[bankB[sb]])
            if tk["cfirst"] == 1:
                for E in range(NCV1, 32):
                    if ((E - NCV1) * 60) // (32 - NCV1) == u:
                        conv_expert(E, [sop])

        def fin_A(tk, a):
            ob = 3 + 2 * tk["pair"] + a
            P.add("vector", lambda e, a=a, ob=ob: e.tensor_copy(out=OT[a], in_=bk(ob)[0:65, :]), reads=[bankB[ob]], writes=[BOT[a]])

        def fin_B(tk, a):
            u, c = tk["u"], tk["c"]
            b, uu, moba, slot = unit_info(u)
            nacc = 1 if moba else 2
            for qt in range(4):
                P.add("tensor", lambda e, a=a, qt=qt: e.transpose(bk(MISC)[:, qt * 65:(qt + 1) * 65], OT[a][0:65, qt * 128:(qt + 1) * 128], ident_f[0:65, 0:65]),
                      reads=[BOT[a], Bc], writes=[bankB[MISC]])
            pf = bk(MISC)[:, 0:260].rearrange("p (q d) -> p q d", d=65)
            P.add("vector", lambda e, pf=pf: e.reciprocal(out=rc[:, 0:4], in_=pf[:, :, 64]), reads=[bankB[MISC]], writes=[Brc])
            if moba:
                col = uu * 64
            else:
                jj = uu - 8
                col = 512 + (jj // 2) * 128 + a * 64
            dst = ATT[:, c * 4:(c + 1) * 4, col:col + 64]
            if moba or (uu - 8) % 2 == 0:
                P.add("vector", lambda e, pf=pf, dst=dst: e.tensor_tensor(out=dst, in0=pf[:, :, 0:64], in1=bc(rc[:, 0:4], [128, 4, 64], 2), op=ALU.mult),
                      reads=[bankB[MISC], Brc], writes=[BATT[c]])
            else:
                P.add("vector", lambda e: e.tensor_scalar(out=rc[:, 4:8], in0=rc[:, 0:4], scalar1=neglam, scalar2=None, op0=ALU.mult),
                      reads=[Brc, Blam], writes=[Brc])
                P.add("vector", lambda e, pf=pf: e.tensor_tensor(out=ftmp, in0=pf[:, :, 0:64], in1=bc(rc[:, 4:8], [128, 4, 64], 2), op=ALU.mult),
                      reads=[bankB[MISC], Brc], writes=[Bftmp])
                P.add("vector", lambda e, dst=dst: e.tensor_tensor(out=dst, in0=dst, in1=ftmp, op=ALU.add), reads=[Bftmp, BATT[c]], writes=[BATT[c]])
            if uu == 15 and a == nacc - 1:
                P.add("sync", lambda e: e.dma_start(out=att_s[b * SEQ + c * 512:b * SEQ + (c + 1) * 512, :].rearrange("(t p) d -> p t d", p=128), in_=ATT[:, c * 4:(c + 1) * 4, :]),
                      reads=[BATT[c]], writes=[Batt_s[b]], dma=True, store=True, join=(c > 0))

        pend = []

        def age_pending(flush=False):
            for it in list(pend):
                tk0 = it[0]
                moba0 = unit_info(tk0["u"])[2]
                nacc0 = 1 if moba0 else 2
                while True:
                    it[1] += 1
                    if it[1] == 1:
                        for a in range(nacc0):
                            fin_A(tk0, a)
                    elif it[1] == 3:
                        fin_B(tk0, 0)
                        if nacc0 == 1:
                            pend.remove(it)
                            break
                    elif it[1] == 4 and nacc0 == 2:
                        fin_B(tk0, 1)
                        pend.remove(it)
                        break
                    if not flush:
                        break

        LOOK = 2
        issued = 0
        for ti, tk in enumerate(tasks):
            while issued < min(len(tasks), ti + 1 + LOOK):
                issue_S(issued)
                issued += 1
            u, c, j = tk["u"], tk["c"], tk["j"]
            b, uu, moba, slot = unit_info(u)
            if tk["first"]:
                tk["pair"] = chunk_ctr[0] % 2
                chunk_ctr[0] += 1
            else:
                tk["pair"] = tasks[ti - 1]["pair"]
            pair = tk["pair"]
            sb, lo = tk["sb"], tk["lo"]
            r = ti % NPT
            P.add("scalar", lambda e, sb=sb, lo=lo, r=r: e.activation(out=PT[r][:, lo:512], in_=bk(sb)[:, lo:512], func=AF.Exp),
                  reads=[bankB[sb]], writes=[BPT[r]])
            if tk["o"] is not None:
                P.add("gpsimd", lambda e, lo=lo, r=r: e.tensor_tensor(out=PT[r][:, lo:lo + 128], in0=PT[r][:, lo:lo + 128], in1=tri, op=ALU.mult),
                      reads=[BPT[r], Bc], writes=[BPT[r]])
            nacc = 1 if moba else 2
            for a in range(nacc):
                ob = 3 + 2 * pair + a
                if moba:
                    vcol = uu * 65
                else:
                    vcol = 520 + (((uu - 8) // 2) * 2 + a) * 65
                P.add("tensor", lambda e, ob=ob, vcol=vcol, lo=lo, r=r, j=j, b=b, tk=tk: e.matmul(bk(ob)[0:65, lo:512], lhsT=VS[b % 2][:, j, vcol:vcol + 65], rhs=PT[r][:, lo:512],
                                                                                                   start=tk["first"], stop=tk["last"], skip_group_check=True),
                      reads=[BVS[b % 2], BPT[r]], writes=[bankB[ob]])
            if moba and P2_WARM:
                db = 4 if ti % 2 == 0 else 6
                P.add("tensor", lambda e, db=db, b=b: e.matmul(bk(db)[:, 0:P2_WARM], lhsT=ident_b, rhs=VS[b % 2][:, 0, 0:P2_WARM], start=True, stop=True),
                      reads=[Bc, BVS[b % 2]], writes=[bankB[db]])
            age_pending()
            if tk["last"]:
                pend.append([tk, 0])
        while pend:
            age_pending(flush=True)
        P.barrier()

        M.off = persist_mark
        kmemT = M.alloc([128, 4, 8, 256], BF16)
        vmem = M.alloc([128, 4, 2, 1028], BF16)
        Bkmem = P.buf("kmemT")
        Bvmem = P.buf("vmem")
        w_out_sb = M.alloc([128, 8, D], BF16)
        w_q_sb = M.alloc([128, 8, D], BF16)
        w_o_sb = M.alloc([128, 8, D], BF16)
        Bwo, Bwq, Bwmo = P.buf("w_out"), P.buf("w_mq"), P.buf("w_mo")
        p3_mark = M.off
        w_kv_sb = M.alloc([128, 8, 2048], BF16)
        Bwkv = P.buf("w_kv")
        load_w_bf16(w_kv_sb, w_mem_kv, 2048, Bwkv)
        load_w_bf16(w_out_sb, w_out, D, Bwo)
        load_w_bf16(w_q_sb, w_mem_q, D, Bwq)
        load_w_bf16(w_o_sb, w_mem_o, D, Bwmo)
        gkv = M.alloc([128, D], F32)
        Bgkv = P.buf("gkv")
        P.add("sync", lambda e: e.dma_start(out=gkv, in_=norm_mem_kv[0:1, :].partition_broadcast(128)), writes=[Bgkv], dma=True)
        mt_ = [M.alloc([128, D], F32) for _ in range(2)]
        Bmt = [P.buf("mt%d" % i) for i in range(2)]
        ssm = [M.alloc([128, 4], F32) for _ in range(2)]
        Bssm = [P.buf("ssm%d" % i) for i in range(2)]
        mn = [M.alloc([128, D], BF16) for _ in range(2)]
        Bmn = [P.buf("mn%d" % i) for i in range(2)]
        mnT = [M.alloc([128, 8, 256], BF16) for _ in range(2)]
        BmnT = [P.buf("mnT%d" % i) for i in range(2)]
        junk = M.alloc([128, D], BF16)
        P.add("vector", lambda e: e.memset(vmem, 1.0), writes=[Bvmem])
        def p3a_b(b):
            s2 = b % 2
            for mt in range(2):
                t = b * 2 + mt
                p2 = t % 2
                P.add("sync", lambda e, t=t, p2=p2: e.dma_start(out=mt_[p2], in_=mem[t * 128:(t + 1) * 128, :]), writes=[Bmt[p2]], dma=True)
                rms_stats(mt_[p2], D, ssm[p2], Bmt[p2], Bssm[p2], junk)
                P.add("vector", lambda e, p2=p2: e.scalar_tensor_tensor(out=mn[p2], in0=mt_[p2], scalar=ssm[p2][:, 2:3], in1=gkv, op0=ALU.mult, op1=ALU.mult),
                      reads=[Bmt[p2], Bssm[p2], Bgkv], writes=[Bmn[p2]])
                transposes_bf16(mn[p2], Bmn[p2], p2, mnT[s2][:, :, mt * 128:(mt + 1) * 128], BmnT[s2], evac=("scalar" if mt else "vector"))
            for fc in range(8):
                bnk = 2 + fc % 4
                for kc in range(8):
                    P.add("tensor", lambda e, fc=fc, kc=kc, bnk=bnk, s2=s2: e.matmul(bk(bnk)[:, 0:256], lhsT=w_kv_sb[:, kc, fc * 128:(fc + 1) * 128], rhs=mnT[s2][:, kc, :],
                                                                                   start=(kc == 0), stop=(kc == 7)),
                          reads=[Bwkv, BmnT[s2]], writes=[bankB[bnk]])
                if fc % 2:
                    P.add("scalar", lambda e, fc=fc, bnk=bnk, b=b: e.activation(out=kmemT[:, b, fc, :], in_=bk(bnk)[:, 0:256], func=AF.Copy), reads=[bankB[bnk]], writes=[Bkmem])
                else:
                    P.add("vector", lambda e, fc=fc, bnk=bnk, b=b: e.tensor_copy(out=kmemT[:, b, fc, :], in_=bk(bnk)[:, 0:256]), reads=[bankB[bnk]], writes=[Bkmem])
            for mt in range(2):
                for hf in range(2):
                    bnk = 6 + hf
                    for kc in range(8):
                        P.add("tensor", lambda e, kc=kc, mt=mt, hf=hf, bnk=bnk, s2=s2: e.matmul(bk(bnk), lhsT=mnT[s2][:, kc, mt * 128:(mt + 1) * 128],
                                                                                              rhs=w_kv_sb[:, kc, 1024 + hf * 512:1024 + (hf + 1) * 512], start=(kc == 0), stop=(kc == 7)),
                              reads=[Bwkv, BmnT[s2]], writes=[bankB[bnk]])
                    dstv = vmem[:, b, mt, hf * 514:(hf + 1) * 514].rearrange("p (h c) -> p h c", c=257)[:, :, 0:256]
                    srcv = bk(bnk).rearrange("p (h c) -> p h c", c=256)
                    if hf:
                        P.add("scalar", lambda e, dstv=dstv, srcv=srcv: e.activation(out=dstv, in_=srcv, func=AF.Copy), reads=[bankB[bnk]], writes=[Bvmem])
                    else:
                        P.add("vector", lambda e, dstv=dstv, srcv=srcv: e.tensor_copy(out=dstv, in_=srcv), reads=[bankB[bnk]], writes=[Bvmem])

        for b in range(4):
            p3a_b(b)
        P.barrier()

        M.off = p3_mark
        w_r = M.alloc([128, 8, 36], F32)
        b_r = M.alloc([128, 36], F32)
        gmoba = M.alloc([128, 512], F32)
        gsub = M.alloc([128, 128], F32)
        gmq = M.alloc([128, D], F32)
        gffn = M.alloc([128, D], F32)
        Bv3 = P.buf("p3vec")
        vl = [(w_r[:, :, 0:4], w_rg.rearrange("(k p) g -> p k g", p=128)), (w_r[:, :, 4:36], w_re.rearrange("(k p) g -> p k g", p=128)),
              (b_r[:, 0:4], b_rg[0:1, :].partition_broadcast(128)), (b_r[:, 4:36], b_re[0:1, :].partition_broadcast(128)),
              (gmoba, norm_moba[0:1, :].partition_broadcast(128)), (gsub, subln[0:1, :].partition_broadcast(128)),
              (gmq, norm_mem_q[0:1, :].partition_broadcast(128)), (gffn, norm_ffn[0:1, :].partition_broadcast(128))]
        for k, (dst, src) in enumerate(vl):
            P.add("sync", lambda e, dst=dst, src=src: e.dma_start(out=dst, in_=src), writes=[Bv3], dma=True, join=(k > 0))
        P.add("vector", lambda e: e.tensor_scalar(out=gsub, in0=gsub, scalar1=1.0 - lam_init, scalar2=None, op0=ALU.mult), reads=[Bv3], writes=[Bv3])
        NSTR = 2
        TPC = 2
        CW = TPC * 128
        NCH = NTILE // TPC
        RS = range(NSTR)
        at = [M.alloc([128, D], F32) for _ in range(NSTR * TPC)]
        Bat = [P.buf("at%d" % i) for i in range(NSTR * TPC)]
        ssA = [M.alloc([128, 16], F32) for _ in RS]
        BssA = [P.buf("ssA%d" % i) for i in RS]
        mixb = [M.alloc([128, D], BF16) for _ in RS]
        Bmixb = [P.buf("mixb%d" % i) for i in RS]
        TT = [M.alloc([128, 8, CW], BF16) for _ in RS]
        BTT = [[P.buf("TT%d_%d" % (s_, i)) for i in range(TPC)] for s_ in RS]
        hres = [M.alloc([128, TPC, D], F32) for _ in RS]
        Bh = [[P.buf("hres%d_%d" % (s_, i)) for i in range(TPC)] for s_ in RS]
        ssB = [M.alloc([128, 4], F32) for _ in RS]
        BssB = [P.buf("ssB%d" % i) for i in RS]
        qmT = [M.alloc([128, 8, CW], BF16) for _ in RS]
        BqmT = [P.buf("qmT%d" % i) for i in RS]
        PmT = [M.alloc([128, 8, CW], BF16) for _ in RS]
        BPm = [[P.buf("PmT%d_%d" % (s_, i)) for i in range(8)] for s_ in RS]
        ca = [M.alloc([128, TPC, D], BF16) for _ in RS]
        Bca = [[P.buf("ca%d_%d" % (s_, i)) for i in range(TPC)] for s_ in RS]
        rc3 = [M.alloc([128, 4], F32) for _ in RS]
        Brc3 = [P.buf("rc3_%d" % i) for i in RS]
        xf = [M.alloc([128, D], F32) for _ in RS]
        Bxf = [P.buf("xf%d" % i) for i in RS]
        xfT = [M.alloc([128, 8, 128], F32) for _ in RS]
        BxfT = [P.buf("xfT%d" % i) for i in RS]
        junk = M.alloc([128, D], BF16)
        lg = [M.alloc([128, TPC, 36], F32) for _ in RS]
        rt = [M.alloc([128, 192], F32) for _ in RS]
        Brt = [P.buf("rt%d" % i) for i in RS]
        Bh2s = P.buf("h2_s")
        Bxns = P.buf("xn_s")
        mmb = [0]
        nst = [0, 0]

        def nxt_bank():
            bnk = 2 + mmb[0] % (3 if P3_WARM else 4)
            mmb[0] += 1
            return bnk

        def p3_warm():
            for _ in range(P3_WARM):
                o = P.add("tensor", lambda e: e.matmul(bk(5), lhsT=ident_b, rhs=w_out_sb[:, 0, 0:512], start=True, stop=True),
                          reads=[Bc, Bwo], writes=[bankB[5]])
                o.pen = P3_WARM_PEN

        def dense_out(srcT, BsrcT, w_sb, Bw, i, hr, Bhr):
            for hf in range(2):
                bnk = nxt_bank()
                for kc in range(8):
                    P.add("tensor", lambda e, kc=kc, hf=hf, bnk=bnk: e.matmul(bk(bnk), lhsT=srcT[:, kc, i * 128:(i + 1) * 128], rhs=w_sb[:, kc, hf * 512:(hf + 1) * 512],
                                                                               start=(kc == 0), stop=(kc == 7)),
                          reads=[BsrcT, Bw], writes=[bankB[bnk]])
                P.add("vector", lambda e, hf=hf, bnk=bnk: e.tensor_tensor(out=hr[:, hf * 512:(hf + 1) * 512], in0=bk(bnk), in1=hr[:, hf * 512:(hf + 1) * 512], op=ALU.add),
                      reads=[bankB[bnk], Bhr], writes=[Bhr])

        def p3_stage(ck, st_):
            s_ = ck % NSTR
            b = (ck * CW) // SEQ
            if st_ == -1:
                for i in range(TPC):
                    t = ck * TPC + i
                    ai = s_ * TPC + i
                    P.add("sync", lambda e, t=t, ai=ai: e.dma_start(out=at[ai], in_=att_s[t * 128:(t + 1) * 128, :]), reads=[Batt_s[b]], writes=[Bat[ai]], dma=True)
            elif st_ == 0:
                for i in range(TPC):
                    t = ck * TPC + i
                    hr = hres[s_][:, i, :]
                    ai = s_ * TPC + i
                    P.add("sync", lambda e, t=t, hr=hr: e.dma_start(out=hr, in_=x[t * 128:(t + 1) * 128, :]), writes=[Bh[s_][i]], dma=True)
                    sa = ssA[s_]
                    P.add("scalar", lambda e, ai=ai, sa=sa, junk=junk: e.activation(out=junk[:, 0:512], in_=at[ai][:, 0:512], func=AF.Square, accum_out=sa[:, 0:1]),
                          reads=[Bat[ai]], writes=[BssA[s_], jbuf(junk)])
                    for h in range(4):
                        P.add("scalar", lambda e, ai=ai, sa=sa, h=h, junk=junk: e.activation(out=junk[:, 0:128], in_=at[ai][:, 512 + h * 128:512 + (h + 1) * 128], func=AF.Square,
                                                                                 accum_out=sa[:, 1 + h:2 + h]),
                              reads=[Bat[ai]], writes=[BssA[s_], jbuf(junk)])
                    P.add("scalar", lambda e, sa=sa: e.activation(out=sa[:, 5:6], in_=sa[:, 0:1], func=AF.Sqrt, bias=eps_t[:, 0:1], scale=1.0 / 512),
                          reads=[BssA[s_], Bc], writes=[BssA[s_]])
                    P.add("scalar", lambda e, sa=sa: e.activation(out=sa[:, 6:10], in_=sa[:, 1:5], func=AF.Sqrt, bias=eps_t[:, 0:1], scale=1.0 / 128),
                          reads=[BssA[s_], Bc], writes=[BssA[s_]])
                    P.add("vector", lambda e, sa=sa: e.reciprocal(out=sa[:, 10:15], in_=sa[:, 5:10]), reads=[BssA[s_]], writes=[BssA[s_]])
                    P.add("vector", lambda e, ai=ai, sa=sa: e.scalar_tensor_tensor(out=mixb[s_][:, 0:512], in0=at[ai][:, 0:512], scalar=sa[:, 10:11], in1=gmoba, op0=ALU.mult, op1=ALU.mult),
                          reads=[Bat[ai], BssA[s_], Bv3], writes=[Bmixb[s_]])
                    for h in range(4):
                        P.add("vector", lambda e, ai=ai, sa=sa, h=h: e.scalar_tensor_tensor(out=mixb[s_][:, 512 + h * 128:512 + (h + 1) * 128], in0=at[ai][:, 512 + h * 128:512 + (h + 1) * 128],
                                                                                    scalar=sa[:, 11 + h:12 + h], in1=gsub, op0=ALU.mult, op1=ALU.mult),
                              reads=[Bat[ai], BssA[s_], Bv3], writes=[Bmixb[s_]])
                    transposes_bf16(mixb[s_], Bmixb[s_], s_, TT[s_][:, :, i * 128:(i + 1) * 128], BTT[s_][i], evac=("scalar" if i % 2 else "vector"))
                    dense_out(TT[s_], BTT[s_][i], w_out_sb, Bwo, i, hr, Bh[s_][i])
            elif st_ == 1:
                for i in range(TPC):
                    hr = hres[s_][:, i, :]
                    rms_stats(hr, D, ssB[s_], Bh[s_][i], BssB[s_], junk)
                    P.add("vector", lambda e, hr=hr: e.scalar_tensor_tensor(out=mixb[s_], in0=hr, scalar=ssB[s_][:, 2:3], in1=gmq, op0=ALU.mult, op1=ALU.mult),
                          reads=[Bh[s_][i], BssB[s_], Bv3], writes=[Bmixb[s_]])
                    transposes_bf16(mixb[s_], Bmixb[s_], s_, TT[s_][:, :, i * 128:(i + 1) * 128], BTT[s_][i], evac=("scalar" if i % 2 else "vector"))
            elif st_ == 2:
                for fc in range(8):
                    bnk = nxt_bank()
                    for kc in range(8):
                        P.add("tensor", lambda e, fc=fc, kc=kc, bnk=bnk: e.matmul(bk(bnk)[:, 0:CW], lhsT=w_q_sb[:, kc, fc * 128:(fc + 1) * 128], rhs=TT[s_][:, kc, :], start=(kc == 0), stop=(kc == 7)),
                              reads=[Bwq] + BTT[s_], writes=[bankB[bnk]])
                    if fc % 2:
                        P.add("scalar", lambda e, fc=fc, bnk=bnk: e.activation(out=qmT[s_][:, fc, :], in_=bk(bnk)[:, 0:CW], func=AF.Copy, scale=1.0 / 16), reads=[bankB[bnk]], writes=[BqmT[s_]])
                    else:
                        P.add("vector", lambda e, fc=fc, bnk=bnk: e.tensor_scalar(out=qmT[s_][:, fc, :], in0=bk(bnk)[:, 0:CW], scalar1=1.0 / 16, scalar2=None, op0=ALU.mult),
                              reads=[bankB[bnk]], writes=[BqmT[s_]])
            elif st_ == 3:
                for h in range(4):
                    for mt in range(2):
                        bnk = nxt_bank()
                        for hf in range(2):
                            P.add("tensor", lambda e, h=h, mt=mt, hf=hf, bnk=bnk: e.matmul(bk(bnk)[:, 0:CW], lhsT=kmemT[:, b, h * 2 + hf, mt * 128:(mt + 1) * 128], rhs=qmT[s_][:, h * 2 + hf, :],
                                                                                            start=(hf == 0), stop=(hf == 1)),
                                  reads=[Bkmem, BqmT[s_]], writes=[bankB[bnk]])
                        P.add("scalar", lambda e, h=h, mt=mt, bnk=bnk: e.activation(out=PmT[s_][:, h * 2 + mt, :], in_=bk(bnk)[:, 0:CW], func=AF.Exp), reads=[bankB[bnk]], writes=[BPm[s_][h * 2 + mt]])
            elif st_ == 4:
                for i in range(TPC):
                    for h in range(4):
                        bnk = nxt_bank()
                        for mt in range(2):
                            P.add("tensor", lambda e, i=i, h=h, mt=mt, bnk=bnk: e.matmul(bk(bnk)[:, 0:257], lhsT=PmT[s_][:, h * 2 + mt, i * 128:(i + 1) * 128], rhs=vmem[:, b, mt, h * 257:(h + 1) * 257],
                                                                                          start=(mt == 0), stop=(mt == 1)),
                                  reads=[BPm[s_][h * 2 + mt], Bvmem], writes=[bankB[bnk]])
                        P.add("vector", lambda e, h=h, bnk=bnk: e.reciprocal(out=rc3[s_][:, h:h + 1], in_=bk(bnk)[:, 256:257]), reads=[bankB[bnk]], writes=[Brc3[s_]])
                        P.add("vector", lambda e, i=i, h=h, bnk=bnk: e.tensor_scalar(out=ca[s_][:, i, h * 256:(h + 1) * 256], in0=bk(bnk)[:, 0:256], scalar1=rc3[s_][:, h:h + 1], scalar2=None, op0=ALU.mult),
                              reads=[bankB[bnk], Brc3[s_]], writes=[Bca[s_][i]])
            elif st_ == 5:
                for i in range(TPC):
                    transposes_bf16(ca[s_][:, i, :], Bca[s_][i], s_, TT[s_][:, :, i * 128:(i + 1) * 128], BTT[s_][i], evac=("scalar" if i % 2 else "vector"))
                for i in range(TPC):
                    dense_out(TT[s_], BTT[s_][i], w_o_sb, Bwmo, i, hres[s_][:, i, :], Bh[s_][i])
                P.add("gpsimd", lambda e: e.dma_start(out=h2_s[ck * CW:(ck + 1) * CW, :].rearrange("(i p) d -> p i d", p=128), in_=hres[s_]),
                      reads=Bh[s_], writes=[Bh2s], dma=True, store=True, join=(nst[0] > 0))
                nst[0] += 1
            elif st_ == 6:
                lgp = nxt_bank()
                for i in range(TPC):
                    t = ck * TPC + i
                    hr = hres[s_][:, i, :]
                    rms_stats(hr, D, ssB[s_], Bh[s_][i], BssB[s_], junk)
                    P.add("vector", lambda e, hr=hr: e.scalar_tensor_tensor(out=xf[s_], in0=hr, scalar=ssB[s_][:, 2:3], in1=gffn, op0=ALU.mult, op1=ALU.mult),
                          reads=[Bh[s_][i], BssB[s_], Bv3], writes=[Bxf[s_]])
                    P.add("scalar", lambda e: e.activation(out=mixb[s_], in_=xf[s_], func=AF.Copy), reads=[Bxf[s_]], writes=[Bmixb[s_]])
                    P.add("gpsimd", lambda e, t=t: e.dma_start(out=xn_s[t * 128:(t + 1) * 128, :], in_=mixb[s_]), reads=[Bmixb[s_]], writes=[Bxns], dma=True, store=True, join=(nst[1] > 0))
                    nst[1] += 1
                    for hf in range(2):
                        tb = 6 + hf
                        for k4 in range(4):
                            kc = hf * 4 + k4
                            P.add("tensor", lambda e, kc=kc, k4=k4, tb=tb: e.transpose(bk(tb)[:, k4 * 128:(k4 + 1) * 128], xf[s_][:, kc * 128:(kc + 1) * 128], ident_f),
                                  reads=[Bxf[s_], Bc], writes=[bankB[tb]])
                        srcv = bk(tb).rearrange("p (k t) -> p k t", k=4)
                        if hf:
                            P.add("scalar", lambda e, srcv=srcv: e.activation(out=xfT[s_][:, 4:8, :], in_=srcv, func=AF.Copy), reads=[bankB[tb]], writes=[BxfT[s_]])
                        else:
                            P.add("vector", lambda e, srcv=srcv: e.tensor_copy(out=xfT[s_][:, 0:4, :], in_=srcv), reads=[bankB[tb]], writes=[BxfT[s_]])
                    for kc in range(8):
                        P.add("tensor", lambda e, i=i, kc=kc: e.matmul(bk(lgp)[:, i * 36:(i + 1) * 36], lhsT=xfT[s_][:, kc, :], rhs=w_r[:, kc, :], start=(kc == 0), stop=(kc == 7)),
                              reads=[BxfT[s_], Bv3], writes=[bankB[lgp]])
                P.add("vector", lambda e: e.tensor_tensor(out=lg[s_], in0=bk(lgp)[:, 0:36 * TPC].rearrange("p (t c) -> p t c", c=36), in1=bc(b_r, [128, TPC, 36], 1), op=ALU.add),
                      reads=[bankB[lgp], Bv3, Brt[s_]], writes=[Brt[s_]])
            if st_ == 7:
                T_ = TPC
                off = [0]

                def R(*shape):
                    n = int(np.prod(shape))
                    ap = rt[s_][:, off[0]:off[0] + n]
                    off[0] += n
                    if len(shape) == 2:
                        ap = ap.rearrange("p (a b) -> p a b", b=shape[1])
                    elif len(shape) == 3:
                        ap = ap.rearrange("p (a b c) -> p a b c", b=shape[1], c=shape[2])
                    return ap
                lgs = lg[s_]
                gl = lgs[:, :, 0:4]
                el = lgs[:, :, 4:36].rearrange("p t (g i) -> p t g i", i=8)
                gmax, goh, gsh, gex, gsum, gw = R(T_), R(T_, 4), R(T_, 4), R(T_, 4), R(T_), R(T_)
                prod = R(T_, 4, 8)
                esel = R(T_, 8)
                m1, eq1, e2, m2, eq2 = R(T_), R(T_, 8), R(T_, 8), R(T_), R(T_, 8)
                dd, ed, den, w1, w2 = R(T_), R(T_), R(T_), R(T_), R(T_)
                assert off[0] <= 192
                V = lambda fn, **kw: P.add("vector", fn, reads=[Brt[s_], Bv3] + kw.get("r", []), writes=[Brt[s_]] + kw.get("w", []))
                V(lambda e: e.tensor_reduce(out=gmax, in_=gl, axis=AX.X, op=ALU.max))
                V(lambda e: e.tensor_tensor(out=goh, in0=gl, in1=bc(gmax, [128, T_, 4], 2), op=ALU.is_ge))
                V(lambda e: e.tensor_tensor(out=gsh, in0=gl, in1=bc(gmax, [128, T_, 4], 2), op=ALU.subtract))
                P.add("scalar", lambda e: e.activation(out=gex, in_=gsh, func=AF.Exp), reads=[Brt[s_]], writes=[Brt[s_]])
                V(lambda e: e.tensor_reduce(out=gsum, in_=gex, axis=AX.X, op=ALU.add))
                V(lambda e: e.reciprocal(out=gw, in_=gsum))
                V(lambda e: e.tensor_tensor(out=prod, in0=el, in1=bc(goh, [128, T_, 4, 8], 3), op=ALU.mult))
                V(lambda e: e.tensor_reduce(out=esel, in_=prod.rearrange("p t g i -> p t i g"), axis=AX.X, op=ALU.add))
                V(lambda e: e.tensor_reduce(out=m1, in_=esel, axis=AX.X, op=ALU.max))
                V(lambda e: e.tensor_tensor(out=eq1, in0=esel, in1=bc(m1, [128, T_, 8], 2), op=ALU.is_ge))
                V(lambda e: e.scalar_tensor_tensor(out=e2, in0=eq1, scalar=-1e30, in1=esel, op0=ALU.mult, op1=ALU.add))
                V(lambda e: e.tensor_reduce(out=m2, in_=e2, axis=AX.X, op=ALU.max))
                V(lambda e: e.tensor_tensor(out=eq2, in0=e2, in1=bc(m2, [128, T_, 8], 2), op=ALU.is_ge))
                V(lambda e: e.tensor_tensor(out=dd, in0=m2, in1=m1, op=ALU.subtract))
                P.add("scalar", lambda e: e.activation(out=ed, in_=dd, func=AF.Exp), reads=[Brt[s_]], writes=[Brt[s_]])
                V(lambda e: e.tensor_scalar(out=den, in0=ed, scalar1=1.0, scalar2=None, op0=ALU.add))
                V(lambda e: e.reciprocal(out=w1, in_=den))
                V(lambda e: e.tensor_tensor(out=w2, in0=ed, in1=w1, op=ALU.mult))
                tsl = slice(ck * T_, (ck + 1) * T_)
                V(lambda e: e.tensor_tensor(out=c12[:, tsl, 0], in0=w1, in1=gw, op=ALU.mult), w=[BA])
                V(lambda e: e.tensor_tensor(out=c12[:, tsl, 1], in0=w2, in1=gw, op=ALU.mult), w=[BA])
                V(lambda e: e.tensor_tensor(out=A1[:, tsl, :].rearrange("p t (g i) -> p t g i", i=8), in0=bc(goh, [128, T_, 4, 8], 3), in1=bc(eq1, [128, T_, 4, 8], 2), op=ALU.mult), w=[BA])
                V(lambda e: e.tensor_tensor(out=A2[:, tsl, :].rearrange("p t (g i) -> p t g i", i=8), in0=bc(goh, [128, T_, 4, 8], 3), in1=bc(eq2, [128, T_, 4, 8], 2), op=ALU.mult), w=[BA])

        NPAIR = NCH // NSTR
        seqs = []
        for s_ in RS:
            sq = [(s_, -1)]
            for m in range(NPAIR):
                ck = m * NSTR + s_
                sq += [(ck, 0), (ck, 1)]
                if m > 0:
                    sq.append((ck - NSTR, 7))
                sq.append((ck, 2))
                if m + 1 < NPAIR:
                    sq.append((ck + NSTR, -1))
                sq += [(ck, 3), (ck, 4), (ck, 5), (ck, 6)]
            sq.append(((NPAIR - 1) * NSTR + s_, 7))
            seqs.append(sq)
        nsq = len(seqs[0])
        for i in range(nsq + P3_LAG):
            if i < nsq:
                p3_stage(*seqs[0][i])
                p3_warm()
            if 0 <= i - P3_LAG < nsq:
                p3_stage(*seqs[1][i - P3_LAG])
                p3_warm()
        P.barrier()

        M.off = persist_mark
        Asum = M.alloc([128, NTILE, 32], BF16)
        cnt = M.alloc([128, NTILE, 32], F32)
        pfx = [M.alloc([128, NTILE, 32], F32) for _ in range(2)]
        rfull = M.alloc([128, NTILE, 32], F32)
        sm = M.alloc([128, 16, 32], F32)
        big3 = M.alloc([128, NS, 32], F32)
        big3b = M.alloc([128, NS, 32], F32)
        Ef = M.alloc([128, NS], F32)
        posf = M.alloc([128, NTILE, 2], F32)
        Bd = P.buf("disp")
        VD = lambda fn, **kw: P.add("vector", fn, reads=[Bd, BA, Bc] + kw.get("r", []), writes=[Bd] + kw.get("w", []))
        VD(lambda e: e.tensor_tensor(out=Asum, in0=A1, in1=A2, op=ALU.add))
        for q in range(4):
            P.add("tensor", lambda e, q=q: e.matmul(bk(q), lhsT=ones_b, rhs=Asum[:, q * 16:(q + 1) * 16, :].rearrange("p t e -> p (t e)"), start=True, stop=True),
                  reads=[Bd, Bc], writes=[bankB[q]])
            P.add("tensor", lambda e, q=q: e.matmul(bk(4 + q), lhsT=ltri, rhs=Asum[:, q * 16:(q + 1) * 16, :].rearrange("p t e -> p (t e)"), start=True, stop=True),
                  reads=[Bd, Bc], writes=[bankB[4 + q]])
            VD(lambda e, q=q: e.tensor_copy(out=cnt[:, q * 16:(q + 1) * 16, :].rearrange("p t e -> p (t e)"), in_=bk(q)), r=[bankB[q]])
            VD(lambda e, q=q: e.tensor_copy(out=rfull[:, q * 16:(q + 1) * 16, :].rearrange("p t e -> p (t e)"), in_=bk(4 + q)), r=[bankB[4 + q]])
        VD(lambda e: e.tensor_copy(out=pfx[0], in_=cnt))
        cur = 0
        sft = 1
        while sft < NTILE:
            VD(lambda e, cur=cur, sft=sft: e.tensor_copy(out=pfx[1 - cur][:, 0:sft, :], in_=pfx[cur][:, 0:sft, :]))
            VD(lambda e, cur=cur, sft=sft: e.tensor_tensor(out=pfx[1 - cur][:, sft:NTILE, :], in0=pfx[cur][:, sft:NTILE, :], in1=pfx[cur][:, 0:NTILE - sft, :], op=ALU.add))
            cur = 1 - cur
            sft *= 2
        incl = pfx[cur]
        excl = pfx[1 - cur]
        VD(lambda e: e.tensor_tensor(out=excl, in0=incl, in1=cnt, op=ALU.subtract))
        ntot = sm[:, 0, :]
        VD(lambda e: e.tensor_copy(out=ntot, in_=incl[:, NTILE - 1, :]))
        thr = misc[:, 0:32]
        eidx = misc[:, 32:64]
        sT = misc[:, 64:64 + NS]
        big_a = big3[:, 0:32, :]
        VD(lambda e: e.tensor_tensor(out=big_a, in0=bc(ntot, [128, 32, 32], 2), in1=bc(thr, [128, 32, 32], 1), op=ALU.is_gt))
        nsl = sm[:, 1, :]
        VD(lambda e: e.tensor_reduce(out=nsl, in_=big_a, axis=AX.X, op=ALU.add))
        sc = [sm[:, 2, :], sm[:, 3, :]]
        VD(lambda e: e.tensor_copy(out=sc[0], in_=nsl))
        cur = 0
        sft = 1
        while sft < 32:
            VD(lambda e, cur=cur, sft=sft: e.tensor_copy(out=sc[1 - cur][:, 0:sft], in_=sc[cur][:, 0:sft]))
            VD(lambda e, cur=cur, sft=sft: e.tensor_tensor(out=sc[1 - cur][:, sft:32], in0=sc[cur][:, sft:32], in1=sc[cur][:, 0:32 - sft], op=ALU.add))
            cur = 1 - cur
            sft *= 2
        inc_e = sc[cur]
        base = sm[:, 4, :]
        endb = sm[:, 5, :]
        VD(lambda e: e.tensor_tensor(out=base, in0=inc_e, in1=nsl, op=ALU.subtract))
        VD(lambda e: e.tensor_scalar(out=base, in0=base, scalar1=float(TSLOT), scalar2=None, op0=ALU.mult))
        VD(lambda e: e.tensor_scalar(out=endb, in0=inc_e, scalar1=float(TSLOT), scalar2=None, op0=ALU.mult))
        VD(lambda e: e.tensor_tensor(out=rfull, in0=rfull, in1=excl, op=ALU.add))
        VD(lambda e: e.tensor_tensor(out=rfull, in0=rfull, in1=bc(base, [128, NTILE, 32], 1), op=ALU.add))
        for k, Ak in enumerate((A1, A2)):
            VD(lambda e, Ak=Ak: e.tensor_tensor(out=cnt, in0=rfull, in1=Ak, op=ALU.mult))
            VD(lambda e, k=k: e.tensor_reduce(out=posf[:, :, k], in_=cnt, axis=AX.X, op=ALU.add))
        VD(lambda e: e.tensor_copy(out=posi, in_=posf), w=[Bpos])
        VD(lambda e: e.tensor_tensor(out=big3, in0=bc(base, [128, NS, 32], 1), in1=bc(sT, [128, NS, 32], 2), op=ALU.is_le))
        VD(lambda e: e.tensor_tensor(out=big3b, in0=bc(endb, [128, NS, 32], 1), in1=bc(sT, [128, NS, 32], 2), op=ALU.is_gt))
        VD(lambda e: e.tensor_tensor(out=big3, in0=big3, in1=big3b, op=ALU.mult))
        VD(lambda e: e.tensor_tensor(out=big3, in0=big3, in1=bc(eidx, [128, NS, 32], 1), op=ALU.mult))
        VD(lambda e: e.tensor_reduce(out=Ef, in_=big3, axis=AX.X, op=ALU.add))
        VD(lambda e: e.tensor_scalar(out=widx, in0=Ef, scalar1=128.0, scalar2=misc[:, 160:161], op0=ALU.mult, op1=ALU.add), w=[Bpos])
        if debug:
            P.add("sync", lambda e: e.dma_start(out=dbg_s[:, 0:128], in_=posf.rearrange("p t k -> p (t k)")), reads=[Bd], writes=[P.buf("dbg")], dma=True)
            P.add("sync", lambda e: e.dma_start(out=dbg_s[:, 128:128 + NS], in_=Ef), reads=[Bd], writes=[P.buf("dbg2")], dma=True)
            P.add("sync", lambda e: e.dma_start(out=dbg_s[:, 256:384], in_=c12.rearrange("p t k -> p (t k)")), reads=[BA], writes=[P.buf("dbg3")], dma=True)
        xg = [M.alloc([128, D], BF16) for _ in range(6)]
        Bxg = [P.buf("xg%d" % i) for i in range(6)]
        Bxs = P.buf("xs_s")
        for t in range(NTILE):
            r3 = t % 6
            P.add("sync", lambda e, t=t, r3=r3: e.dma_start(out=xg[r3], in_=xn_s[t * 128:(t + 1) * 128, :]), reads=[Bxns], writes=[Bxg[r3]], dma=True)
            for k in range(2):
                P.add("gpsimd", lambda e, t=t, r3=r3, k=k: e.indirect_dma_start(out=xs_s[:, :], out_offset=bass.IndirectOffsetOnAxis(ap=posi[:, t, k:k + 1], axis=0),
                                                                                in_=xg[r3], in_offset=None),
                      reads=[Bxg[r3], Bpos], writes=[Bxs], dma=True, store=True, join=(t + k > 0))

        slot_mark = M.off
        Wgu = [M.alloc([128, 8, 1024], BF16) for _ in range(2)]
        Wd = [M.alloc([128, 4, D], BF16) for _ in range(2)]
        BW = [P.buf("W%d" % i) for i in range(2)]
        xsl = [M.alloc([128, 2, D], BF16) for _ in range(3)]
        Bxsl = [P.buf("xsl%d" % i) for i in range(3)]
        xT = [M.alloc([128, 8, 256], BF16) for _ in range(2)]
        BxT = [P.buf("xT%d" % i) for i in range(2)]
        sg = [M.alloc([128, 256], F32) for _ in range(2)]
        Bsg = [P.buf("sg%d" % i) for i in range(2)]
        hT = [M.alloc([128, 4, 256], BF16) for _ in range(2)]
        BhT = [P.buf("hT%d" % i) for i in range(2)]
        ys = [M.alloc([128, 2, D], BF16) for _ in range(2)]
        Bys = [P.buf("ys%d" % i) for i in range(2)]
        Bys_s = P.buf("y_s")

        def slot_loads_x(s):
            s3 = s % 3
            P.add("sync", lambda e: e.dma_start(out=xsl[s3], in_=xs_s[s * TSLOT:(s + 1) * TSLOT, :].rearrange("(i p) d -> p i d", p=128)),
                  reads=[Bxs], writes=[Bxsl[s3]], dma=True)

        def slot_loads(s):
            s2 = s % 2
            P.add("gpsimd", lambda e: e.indirect_dma_start(out=Wgu[s2].rearrange("p k f -> p (k f)"), out_offset=None, in_=wgu_b[:, :],
                                                           in_offset=bass.IndirectOffsetOnAxis(ap=widx[:, s:s + 1], axis=0)),
                  reads=[Bpos, Bwcv], writes=[BW[s2]], dma=True)
            P.add("gpsimd", lambda e: e.indirect_dma_start(out=Wd[s2].rearrange("p k f -> p (k f)"), out_offset=None, in_=wd_b[:, :],
                                                           in_offset=bass.IndirectOffsetOnAxis(ap=widx[:, s:s + 1], axis=0)),
                  reads=[Bpos, Bwcv], writes=[BW[s2]], dma=True, join=True)

        def slot_T(s):
            s2 = s % 2
            s3 = s % 3
            for i in range(2):
                transposes_bf16(xsl[s3][:, i, :], Bxsl[s3], i, xT[s2][:, :, i * 128:(i + 1) * 128], BxT[s2], evac=("scalar" if i else "vector"))

        def slot_body(s):
            s2 = s % 2
            if s + 2 < NS:
                slot_loads_x(s + 2)
            if s + 1 < NS:
                slot_loads(s + 1)
            for fc in range(4):
                gb = 2 + (fc % 2) * 2
                ub = gb + 1
                for kc in range(8):
                    P.add("tensor", lambda e, fc=fc, kc=kc, gb=gb: e.matmul(bk(gb)[:, 0:256], lhsT=Wgu[s2][:, kc, fc * 128:(fc + 1) * 128], rhs=xT[s2][:, kc, :], start=(kc == 0), stop=(kc == 7)),
                          reads=[BW[s2], BxT[s2]], writes=[bankB[gb]])
                for kc in range(8):
                    P.add("tensor", lambda e, fc=fc, kc=kc, ub=ub: e.matmul(bk(ub)[:, 0:256], lhsT=Wgu[s2][:, kc, 512 + fc * 128:512 + (fc + 1) * 128], rhs=xT[s2][:, kc, :], start=(kc == 0), stop=(kc == 7)),
                          reads=[BW[s2], BxT[s2]], writes=[bankB[ub]])
                f2 = fc % 2
                P.add("scalar", lambda e, gb=gb, f2=f2: e.activation(out=sg[f2], in_=bk(gb)[:, 0:256], func=AF.Silu), reads=[bankB[gb]], writes=[Bsg[f2]])
                P.add("vector", lambda e, fc=fc, ub=ub, f2=f2: e.tensor_tensor(out=hT[s2][:, fc, :], in0=sg[f2], in1=bk(ub)[:, 0:256], op=ALU.mult),
                      reads=[Bsg[f2], bankB[ub]], writes=[BhT[s2]])
            if s + 1 < NS:
                slot_T(s + 1)
            for i in range(2):
                for hf in range(2):
                    ob = 6 + hf
                    for fc in range(4):
                        P.add("tensor", lambda e, i=i, hf=hf, fc=fc, ob=ob: e.matmul(bk(ob), lhsT=hT[s2][:, fc, i * 128:(i + 1) * 128], rhs=Wd[s2][:, fc, hf * 512:(hf + 1) * 512],
                                                                                      start=(fc == 0), stop=(fc == 3)),
                              reads=[BhT[s2], BW[s2]], writes=[bankB[ob]])
                    if hf:
                        P.add("scalar", lambda e, i=i, hf=hf, ob=ob: e.activation(out=ys[s2][:, i, hf * 512:(hf + 1) * 512], in_=bk(ob), func=AF.Copy), reads=[bankB[ob]], writes=[Bys[s2]])
                    else:
                        P.add("vector", lambda e, i=i, hf=hf, ob=ob: e.tensor_copy(out=ys[s2][:, i, hf * 512:(hf + 1) * 512], in_=bk(ob)), reads=[bankB[ob]], writes=[Bys[s2]])
            P.add("sync", lambda e, s=s: e.dma_start(out=y_s[s * TSLOT:(s + 1) * TSLOT, :].rearrange("(i p) d -> p i d", p=128), in_=ys[s2]),
                  reads=[Bys[s2]], writes=[Bys_s], dma=True, store=True, join=(s > 0))

        slot_loads_x(0)
        slot_loads_x(1)
        slot_loads(0)
        slot_T(0)
        for s in range(NS):
            slot_body(s)
        P.barrier()

        M.off = slot_mark
        gfin = M.alloc([128, D], F32)
        Bgf = P.buf("gfin")
        P.add("sync", lambda e: e.dma_start(out=gfin, in_=norm_final[0:1, :].partition_broadcast(128)), writes=[Bgf], dma=True)
        NB4 = 6
        y1 = [M.alloc([128, D], BF16) for _ in range(NB4)]
        y2 = [M.alloc([128, D], BF16) for _ in range(NB4)]
        h2t = [M.alloc([128, D], F32) for _ in range(NB4)]
        acc = [M.alloc([128, D], F32) for _ in range(2)]
        ot = [M.alloc([128, D], F32) for _ in range(3)]
        ssF = [M.alloc([128, 4], F32) for _ in range(2)]
        By1 = [P.buf("y1_%d" % i) for i in range(NB4)]
        By2 = [P.buf("y2_%d" % i) for i in range(NB4)]
        Bh2t = [P.buf("h2t%d" % i) for i in range(NB4)]
        Bacc = [P.buf("acc%d" % i) for i in range(2)]
        Bot = [P.buf("ot%d" % i) for i in range(3)]
        BssF = [P.buf("ssF%d" % i) for i in range(2)]
        Bout = P.buf("out")
        junk = M.alloc([128, D], BF16)

        def fin_load(t):
            p4 = t % NB4
            P.add("sync", lambda e, t=t, p4=p4: e.dma_start(out=h2t[p4], in_=h2_s[t * 128:(t + 1) * 128, :]), reads=[Bh2s], writes=[Bh2t[p4]], dma=True)
            for k, (yy, By) in enumerate(((y1, By1), (y2, By2))):
                P.add("gpsimd", lambda e, t=t, p4=p4, k=k, yy=yy: e.indirect_dma_start(out=yy[p4], out_offset=None, in_=y_s[:, :],
                                                                                       in_offset=bass.IndirectOffsetOnAxis(ap=posi[:, t, k:k + 1], axis=0)),
                      reads=[Bys_s, Bpos], writes=[By[p4]], dma=True)

        def fin_tile(t):
            p2 = t % 2
            p3 = t % 3
            p4 = t % NB4
            P.add("vector", lambda e, t=t, p2=p2, p4=p4: e.scalar_tensor_tensor(out=acc[p2], in0=y1[p4], scalar=c12[:, t, 0:1], in1=h2t[p4], op0=ALU.mult, op1=ALU.add),
                  reads=[By1[p4], Bh2t[p4], BA], writes=[Bacc[p2]])
            P.add("vector", lambda e, t=t, p2=p2, p4=p4: e.scalar_tensor_tensor(out=acc[p2], in0=y2[p4], scalar=c12[:, t, 1:2], in1=acc[p2], op0=ALU.mult, op1=ALU.add),
                  reads=[By2[p4], Bacc[p2], BA], writes=[Bacc[p2]])
            rms_stats(acc[p2], D, ssF[p2], Bacc[p2], BssF[p2], junk)
            P.add("vector", lambda e, p2=p2, p3=p3: e.scalar_tensor_tensor(out=ot[p3], in0=acc[p2], scalar=ssF[p2][:, 2:3], in1=gfin, op0=ALU.mult, op1=ALU.mult),
                  reads=[Bacc[p2], BssF[p2], Bgf], writes=[Bot[p3]])

        def fin_store(t):
            p3 = t % 3
            P.add("sync", lambda e, t=t, p3=p3: e.dma_start(out=out[t * 128:(t + 1) * 128, :], in_=ot[p3]), reads=[Bot[p3]], writes=[Bout], dma=True, store=True, join=(t > 0))

        for t in range(min(NB4 - 1, NTILE)):
            fin_load(t)
        for t in range(NTILE):
            fin_tile(t)
            if t + NB4 - 1 < NTILE:
                fin_load(t + NB4 - 1)
            fin_store(t)
        if SCHED is not None:
            P.schedule(None if SCHED == "all" else SCHED)
        P.emit(st)
    return nc


def _consts():
    bf = ml_dtypes.bfloat16
    c = {}
    c["c_ident"] = np.eye(128, dtype=np.float32)
    c["c_identb"] = np.eye(128, dtype=np.float32).astype(bf)
    k = np.arange(128)
    c["c_tri"] = (k[None, :] >= k[:, None]).astype(np.float32).astype(bf)
    c["c_ltri"] = (k[:, None] < k[None, :]).astype(np.float32).astype(bf)
    pos = np.arange(SEQ)
    km = np.zeros((8, 12, SEQ), np.float32)
    qm = np.zeros((8, 4, SEQ), np.float32)
    for h in range(8):
        slope = 2.0 ** (-8.0 * (h + 1) / 8)
        for bl in range(8):
            km[h, bl, bl * 256:(bl + 1) * 256] = 1.0
        km[h, 8] = slope * 256.0 * (pos // 256)
        km[h, 9] = slope * (pos % 256)
        km[h, 10] = 1.0
        km[h, 11] = 1.0
        qm[h, 0] = 1.0
        qm[h, 1] = 1.0
        qm[h, 2] = -slope * 256.0 * (pos // 256)
        qm[h, 3] = -slope * (pos % 256)
    c["c_kmoba"] = km.astype(bf)
    c["c_qmoba"] = qm.astype(bf)
    kd = np.zeros((4, 4, SEQ), np.float32)
    qd = np.zeros((4, 4, SEQ), np.float32)
    for h in range(4):
        slope = 2.0 ** (-8.0 * (h + 1) / 4)
        kd[h, 0] = slope * 256.0 * (pos // 256)
        kd[h, 1] = slope * (pos % 256)
        kd[h, 2] = 1.0
        kd[h, 3] = 1.0
        qd[h, 0] = 1.0
        qd[h, 1] = 1.0
        qd[h, 2] = -slope * 256.0 * (pos // 256)
        qd[h, 3] = -slope * (pos % 256)
    c["c_kdiff"] = kd.astype(bf)
    c["c_qdiff"] = qd.astype(bf)
    past = np.zeros((2, 16, 8), np.float32)
    for t in range(16):
        own = t // 2
        for bl in range(8):
            past[0, t, bl] = 0.0 if bl < own else -1e30
            past[1, t, bl] = 1.0 if bl < own else 0.0
    c["c_past"] = past.reshape(2, 128)
    misc = np.zeros((128, 256), np.float32)
    misc[:, 0:32] = 256.0 * np.arange(32)[None, :]
    misc[:, 32:64] = np.arange(32)[None, :]
    misc[:, 64:64 + NS] = 256.0 * np.arange(NS)[None, :]
    p = np.arange(128)[:, None]
    misc[:, 160:168] = np.arange(8)[None, :] * 128 + p
    misc[:, 168:172] = np.arange(4)[None, :] * 128 + p
    c["c_misc"] = misc
    return c


_CACHE = {}


def kernel(x, mem, norm_mix, w_in, lambda_q1, lambda_k1, lambda_q2, lambda_k2, diff_subln,
           norm_moba_out, w_out, norm_mem_q, norm_mem_kv, w_mem_q, w_mem_kv, w_mem_o,
           norm_ffn, w_router_group, b_router_group, w_router_expert, b_router_expert,
           w_expert_gate, w_expert_up, w_expert_down, norm_final, _debug=False):
    f = lambda a: np.ascontiguousarray(np.asarray(a, dtype=np.float32))
    x = f(x)
    mem = f(mem)
    shared = dict(
        w_in=f(w_in)[0], w_out=f(w_out)[0], w_mem_q=f(w_mem_q)[0], w_mem_kv=f(w_mem_kv)[0], w_mem_o=f(w_mem_o)[0],
        w_rg=f(w_router_group)[0], w_re=f(w_router_expert)[0], b_rg=f(b_router_group).reshape(1, 4), b_re=f(b_router_expert).reshape(1, 32),
        w_gate=f(w_expert_gate)[0].reshape(32 * D, 512), w_up=f(w_expert_up)[0].reshape(32 * D, 512), w_down=f(w_expert_down)[0].reshape(32 * 512, D),
        norm_mix=f(norm_mix).reshape(1, D), norm_moba=f(norm_moba_out).reshape(1, 512), subln=f(diff_subln).reshape(1, 128),
        norm_mem_q=f(norm_mem_q).reshape(1, D), norm_mem_kv=f(norm_mem_kv).reshape(1, D), norm_ffn=f(norm_ffn).reshape(1, D),
        norm_final=f(norm_final).reshape(1, D),
        lam4=np.concatenate([f(lambda_q1).reshape(1, 64), f(lambda_k1).reshape(1, 64), f(lambda_q2).reshape(1, 64), f(lambda_k2).reshape(1, 64)], axis=0),
    )
    shared.update(_consts())
    key = bool(_debug)
    if key not in _CACHE:
        _CACHE[key] = build(debug=_debug)
    nc = _CACHE[key]
    in_maps = []
    for c in range(NCORES):
        m = dict(shared)
        m["x"] = x[4 * c:4 * c + 4].reshape(NTOK, D)
        m["mem"] = mem[4 * c:4 * c + 4].reshape(1024, D)
        in_maps.append(m)
    res = run_bass_kernel_spmd(nc, in_maps, core_ids=list(range(NCORES)))
    if _debug:
        return res
    outp = np.concatenate([np.asarray(r["out"]).reshape(4, SEQ, D) for r in res.results], axis=0)
    return outp.astype(np.float32)
```
